# Optimizing a Trainium2 kernel written in Bass

```python
import jax
import jax.numpy as jnp
from jax import lax
import numpy as np

D_MODEL = 1024
BATCH = 32
SEQ = 2048
DEPTH = 2

GRID_W = 64
CTX_LEN = 256
N_MIXERS = 2
EPS = 1e-6

GLA_HEADS = 4
GLA_KDIM = D_MODEL // 2
GLA_VDIM = D_MODEL
GLA_KHEAD = GLA_KDIM // GLA_HEADS
GLA_VHEAD = GLA_VDIM // GLA_HEADS
GLA_GATE_RANK = 16
GLA_GATE_NORM = 16.0
GLA_CHUNK = 64

POOL_WINDOWS = (2, 4, 8, 16)
POOL_GROUP = D_MODEL // len(POOL_WINDOWS)

N_EXPERTS = 16
N_EXPERT_GROUPS = 4
EXPERTS_PER_GROUP = N_EXPERTS // N_EXPERT_GROUPS
TOP_K = 2
D_EXPERT = D_MODEL // 2

kernel_name = 'hybrid_gla_pool_moe_prefix_dit'


def _rmsnorm(x, g):
    xf = x.astype(jnp.float32)
    y = xf * lax.rsqrt(jnp.mean(xf * xf, axis=-1, keepdims=True) + EPS)
    return (y * g.astype(jnp.float32)).astype(x.dtype)


def _modulate(h, shift, scale):
    return h * (1 + scale) + shift


def _gla_scan(q, k, v, g, s0, with_outputs):
    bsz, nh, t, _ = q.shape
    dv = v.shape[-1]
    n = t // GLA_CHUNK

    def chunks(a):
        a = a.astype(jnp.float32).reshape(bsz, nh, n, GLA_CHUNK, a.shape[-1])
        return jnp.moveaxis(a, 2, 0)

    lower = jnp.tril(jnp.ones((GLA_CHUNK, GLA_CHUNK), dtype=bool))[:, :, None]

    def step(s, inp):
        qc, kc, vc, gc = inp
        cum = jnp.cumsum(gc, axis=2)
        last = cum[:, :, -1:, :]
        s_new = jnp.exp(last[:, :, 0, :])[..., None] * s + jnp.einsum(
            'bhjd,bhje->bhde', kc * jnp.exp(last - cum), vc)
        if not with_outputs:
            return s_new, None
        diff = cum[:, :, :, None, :] - cum[:, :, None, :, :]
        decay = jnp.exp(jnp.where(lower, diff, -jnp.inf))
        scores = jnp.einsum('bhid,bhjd,bhijd->bhij', qc, kc, decay)
        o = jnp.einsum('bhid,bhde->bhie', qc * jnp.exp(cum), s) + jnp.einsum(
            'bhij,bhje->bhie', scores, vc)
        return s_new, o

    s_fin, o = lax.scan(step, s0, (chunks(q), chunks(k), chunks(v), chunks(g)))
    if not with_outputs:
        return None, s_fin
    o = jnp.moveaxis(o, 0, 2).reshape(bsz, nh, t, dv)
    return o, s_fin


def _gla_project(h, w_in, w_ga, w_gb, b_g):
    bsz, t, _ = h.shape
    proj = h @ w_in
    q, k, v, r = jnp.split(proj, [GLA_KDIM, 2 * GLA_KDIM, 2 * GLA_KDIM + GLA_VDIM], axis=-1)

    def heads(a, d):
        return a.reshape(bsz, t, GLA_HEADS, d).transpose(0, 2, 1, 3)

    q = heads(q, GLA_KHEAD) * (GLA_KHEAD ** -0.5)
    k = heads(k, GLA_KHEAD)
    v = heads(v, GLA_VHEAD)
    low = jnp.einsum('btd,zdr->zbtr', h, w_ga)
    z = jnp.einsum('zbtr,zrk->zbtk', low, w_gb) + b_g[:, None, None, :]
    logdecay = jax.nn.log_sigmoid(z.astype(jnp.float32)) / GLA_GATE_NORM
    logdecay = logdecay.reshape(2, bsz, t, GLA_HEADS, GLA_KHEAD).transpose(0, 1, 3, 2, 4)
    return q, k, v, r, logdecay


def _gla_readout(o, r, norm_g, w_out):
    bsz, _, t, _ = o.shape
    o = o * lax.rsqrt(jnp.mean(o * o, axis=-1, keepdims=True) + EPS) * norm_g.astype(jnp.float32)
    o = o.transpose(0, 2, 1, 3).reshape(bsz, t, GLA_VDIM).astype(r.dtype)
    return (o * jax.nn.silu(r)) @ w_out


def _gla_mixer(h_lat, h_ctx, w_in, w_ga, w_gb, b_g, norm_g, w_out, ctx_out):
    ql, kl, vl, rl, gl = _gla_project(h_lat, w_in, w_ga, w_gb, b_g)
    qc, kc, vc, rc, gc = _gla_project(h_ctx, w_in, w_ga, w_gb, b_g)

    def flip(a):
        return jnp.flip(a, axis=2)

    s0 = jnp.zeros((h_lat.shape[0], GLA_HEADS, GLA_KHEAD, GLA_VHEAD), jnp.float32)
    oc_f, sc_f = _gla_scan(qc, kc, vc, gc[0], s0, ctx_out)
    oc_b, sc_b = _gla_scan(flip(qc), flip(kc), flip(vc), flip(gc[1]), s0, ctx_out)
    ol_f, _ = _gla_scan(ql, kl, vl, gl[0], sc_f, True)
    ol_b, _ = _gla_scan(flip(ql), flip(kl), flip(vl), flip(gl[1]), sc_b, True)
    y_lat = _gla_readout(ol_f + flip(ol_b), rl, norm_g, w_out)
    y_ctx = _gla_readout(oc_f + flip(oc_b), rc, norm_g, w_out) if ctx_out else None
    return y_lat, y_ctx


def _pool_mixer(h, w_pool, b_pool, scale):
    s, l, _ = h.shape
    hf = h.astype(jnp.float32).reshape(s, l, len(POOL_WINDOWS), POOL_GROUP)
    csum = jnp.concatenate([jnp.zeros_like(hf[:, :1]), jnp.cumsum(hf, axis=1)], axis=1)
    pos = np.arange(l)
    outs = []
    for gi, w in enumerate(POOL_WINDOWS):
        lo = np.clip(pos - w // 2, 0, l)
        hi = np.clip(pos - w // 2 + w, 0, l)
        cnt = (hi - lo).astype(np.float32)
        mean = (csum[:, hi, gi] - csum[:, lo, gi]) / cnt[None, :, None]
        outs.append(mean - hf[:, :, gi])
    pooled = jnp.stack(outs, axis=2).astype(h.dtype)
    y = jnp.einsum('slgc,gce->slge', pooled, w_pool) + b_pool
    return y.reshape(s, l, D_MODEL) * scale


def _moe(h, w_router, b_router, w1, w3, w2):
    n = h.shape[0]
    scores = jax.nn.sigmoid((h @ w_router).astype(jnp.float32))
    sel = scores + b_router.astype(jnp.float32)
    grp = sel.reshape(n, N_EXPERT_GROUPS, EXPERTS_PER_GROUP)
    grp_score = jnp.sum(lax.top_k(grp, TOP_K)[0], axis=-1)
    best = jnp.argmax(grp_score, axis=-1)
    in_group = (jnp.arange(N_EXPERTS) // EXPERTS_PER_GROUP)[None, :] == best[:, None]
    _, idx = lax.top_k(jnp.where(in_group, sel, -jnp.inf), TOP_K)
    wts = jnp.take_along_axis(scores, idx, axis=-1)
    wts = wts / jnp.sum(wts, axis=-1, keepdims=True)
    gates = jnp.sum(jax.nn.one_hot(idx, N_EXPERTS, dtype=jnp.float32) * wts[..., None], axis=1)
    gates = gates.astype(h.dtype)
    out = jnp.zeros_like(h)
    for e in range(N_EXPERTS):
        he = jax.nn.silu(h @ w1[e]) * (h @ w3[e])
        out = out + gates[:, e:e + 1] * (he @ w2[e])
    return out


def setup_inputs(seed: int = 0) -> dict:
    key = jax.random.key(seed)
    ks = jax.random.split(key, 24)
    n_gla = len(range(0, DEPTH, N_MIXERS))
    n_pool = len(range(1, DEPTH, N_MIXERS))
    f32 = jnp.float32

    def nrm(k, shape, fan):
        return jax.random.normal(k, shape, f32) * (fan ** -0.5)

    def rnd(k, shape, s):
        return jax.random.normal(k, shape, f32) * s

    return {
        'x': rnd(ks[0], (BATCH, SEQ, D_MODEL), 1.0),
        'c': rnd(ks[1], (BATCH, D_MODEL), 1.0),
        'ctx': rnd(ks[2], (BATCH, CTX_LEN, D_MODEL), 1.0),
        'c_ctx': rnd(ks[3], (D_MODEL,), 1.0),
        'norm1_g': 1.0 + rnd(ks[4], (DEPTH, D_MODEL), 0.02),
        'norm2_g': 1.0 + rnd(ks[5], (DEPTH, D_MODEL), 0.02),
        'w_mod': 0.5 * nrm(ks[6], (DEPTH, D_MODEL, 6 * D_MODEL), D_MODEL),
        'b_mod': rnd(ks[7], (DEPTH, 6 * D_MODEL), 0.02),
        'gla_w_in': nrm(ks[8], (n_gla, D_MODEL, 2 * GLA_KDIM + 2 * GLA_VDIM), D_MODEL),
        'gla_w_gate_a': nrm(ks[9], (n_gla, 2, D_MODEL, GLA_GATE_RANK), D_MODEL),
        'gla_w_gate_b': nrm(ks[10], (n_gla, 2, GLA_GATE_RANK, GLA_KDIM), GLA_GATE_RANK),
        'gla_b_gate': rnd(ks[11], (n_gla, 2, GLA_KDIM), 0.1),
        'gla_norm_g': 1.0 + rnd(ks[12], (n_gla, GLA_VHEAD), 0.02),
        'gla_w_out': nrm(ks[13], (n_gla, GLA_VDIM, D_MODEL), GLA_VDIM),
        'pool_w': nrm(ks[14], (n_pool, len(POOL_WINDOWS), POOL_GROUP, POOL_GROUP), POOL_GROUP),
        'pool_b': rnd(ks[15], (n_pool, len(POOL_WINDOWS), POOL_GROUP), 0.02),
        'pool_scale': 1.0 + rnd(ks[16], (n_pool, D_MODEL), 0.1),
        'w_router': nrm(ks[17], (D_MODEL, N_EXPERTS), D_MODEL),
        'b_router': rnd(ks[18], (N_EXPERTS,), 0.01),
        'w_gate_e': nrm(ks[19], (DEPTH, N_EXPERTS, D_MODEL, D_EXPERT), D_MODEL),
        'w_up_e': nrm(ks[20], (DEPTH, N_EXPERTS, D_MODEL, D_EXPERT), D_MODEL),
        'w_down_e': nrm(ks[21], (DEPTH, N_EXPERTS, D_EXPERT, D_MODEL), D_EXPERT),
        'final_g': 1.0 + rnd(ks[22], (D_MODEL,), 0.02),
    }


def reference(x, c, ctx, c_ctx, norm1_g, norm2_g, w_mod, b_mod, gla_w_in, gla_w_gate_a,
              gla_w_gate_b, gla_b_gate, gla_norm_g, gla_w_out, pool_w, pool_b, pool_scale,
              w_router, b_router, w_gate_e, w_up_e, w_down_e, final_g):
    bsz, t, _ = x.shape
    rows = t // GRID_W
    n_lat = bsz * t
    for i in range(DEPTH):
        mixer = i % N_MIXERS
        slot = i // N_MIXERS
        ctx_next = any(j % N_MIXERS == 0 for j in range(i + 1, DEPTH))
        use_ctx = mixer == 0 or ctx_next

        mod = jax.nn.silu(c) @ w_mod[i] + b_mod[i]
        sh1, sc1, g1, sh2, sc2, g2 = jnp.split(mod[:, None, :], 6, axis=-1)
        h = _modulate(_rmsnorm(x, norm1_g[i]), sh1, sc1)
        if use_ctx:
            mod_c = jax.nn.silu(c_ctx) @ w_mod[i] + b_mod[i]
            csh1, csc1, cg1, csh2, csc2, cg2 = jnp.split(mod_c, 6, axis=-1)
            hc = _modulate(_rmsnorm(ctx, norm1_g[i]), csh1, csc1)

        if mixer == 0:
            y, yc = _gla_mixer(h, hc, gla_w_in[slot], gla_w_gate_a[slot], gla_w_gate_b[slot],
                               gla_b_gate[slot], gla_norm_g[slot], gla_w_out[slot], ctx_next)
        else:
            y = _pool_mixer(h.reshape(bsz * rows, GRID_W, D_MODEL), pool_w[slot], pool_b[slot],
                            pool_scale[slot]).reshape(bsz, t, D_MODEL)
            yc = _pool_mixer(hc, pool_w[slot], pool_b[slot], pool_scale[slot]) if ctx_next else None

        x = x + g1 * y
        h2 = _modulate(_rmsnorm(x, norm2_g[i]), sh2, sc2).reshape(n_lat, D_MODEL)
        if ctx_next:
            ctx = ctx + cg1 * yc
            h2c = _modulate(_rmsnorm(ctx, norm2_g[i]), csh2, csc2).reshape(-1, D_MODEL)
            f = _moe(jnp.concatenate([h2, h2c], axis=0), w_router, b_router,
                     w_gate_e[i], w_up_e[i], w_down_e[i])
            x = x + g2 * f[:n_lat].reshape(bsz, t, D_MODEL)
            ctx = ctx + cg2 * f[n_lat:].reshape(ctx.shape)
        else:
            f = _moe(h2, w_router, b_router, w_gate_e[i], w_up_e[i], w_down_e[i])
            x = x + g2 * f.reshape(bsz, t, D_MODEL)
    return _rmsnorm(x, final_g)
```

```python
import contextlib
import os
import numpy as np
import ml_dtypes
import concourse.bass as bass
import concourse.mybir as mybir
from concourse.bass_utils import run_bass_kernel_spmd

F32 = mybir.dt.float32
BF16 = mybir.dt.bfloat16
AF = mybir.ActivationFunctionType
ALU = mybir.AluOpType
AX = mybir.AxisListType

SIG_R = 4000
DMA_R = 250
EPS = 1e-6
NCORES = 8
SEQ_PER_CORE = 4
T = 2048
D = 1024
CTX = 256
NE = 16
DE = 512


class Prog:
    ENGS = ("pe", "act", "dve", "pool", "sp")

    def __init__(self, nc):
        self.nc = nc
        self.ops = []
        self.keys = {}
        self.eng_ops = {e: [] for e in self.ENGS}
        self.dma_cnt = {}
        self.last_dma = {}

    def add(self, eng, fn, reads=(), writes=(), dma=None, extra_deps=None):
        idx = len(self.ops)
        deps = {}
        psk = {("PS", k[1]) for k in list(reads) + list(writes) if isinstance(k, tuple) and k[0] == "PS"}
        if psk:
            reads = [k for k in reads if not (isinstance(k, tuple) and k[0] == "PS")]
            writes = [k for k in writes if not (isinstance(k, tuple) and k[0] == "PS")] + sorted(psk)
        for k in reads:
            st = self.keys.get(k)
            if st is not None and st[0] is not None:
                deps.setdefault(st[0], "raw")
        for k in writes:
            st = self.keys.get(k)
            if st is not None:
                if st[0] is not None and st[0] not in deps:
                    deps[st[0]] = "waw"
                for r in st[1]:
                    if r not in deps:
                        deps[r] = "war"
        if extra_deps:
            for d in extra_deps:
                deps[d] = "raw"
        if dma is not None and dma in self.last_dma:
            deps[self.last_dma[dma]] = "raw"
        for k in reads:
            st = self.keys.setdefault(k, [None, []])
            if dma is None:
                st[1] = [r for r in st[1] if not (self.ops[r]["dma"] is None and self.ops[r]["eng"] == eng)]
            st[1].append(idx)
        for k in writes:
            self.keys[k] = [idx, []]
        dcount = None
        if dma is not None:
            dcount = self.dma_cnt.get(dma, 0)
            self.dma_cnt[dma] = dcount + 1
            self.last_dma[dma] = idx
        op = dict(eng=eng, fn=fn, deps=deps, dma=dma, dcount=dcount, seq=len(self.eng_ops[eng]),
                  waits=[], signal=False)
        self.ops.append(op)
        self.eng_ops[eng].append(idx)
        return idx

    def barrier(self):
        last = {}
        for e in self.ENGS:
            for i in reversed(self.eng_ops[e]):
                if self.ops[i]["dma"] is None and self.ops[i]["fn"] is not None:
                    last[e] = i
                    break
        dl = list(self.last_dma.values())
        for e in self.ENGS:
            deps = [i for (d, i) in last.items() if d != e] + dl
            self.add(e, None, extra_deps=deps)

    def finalize_and_emit(self):
        nc = self.nc
        ops = self.ops
        waited_seq = {e: {d: -1 for d in self.ENGS} for e in self.ENGS}
        waited_dma = {e: {} for e in self.ENGS}
        for i, op in enumerate(ops):
            E = op["eng"]
            for d, typ in op["deps"].items():
                dop = ops[d]
                if dop["dma"] is not None:
                    key = (dop["dma"], dop["dcount"] // DMA_R)
                    val = dop["dcount"] % DMA_R + 1
                    if waited_dma[E].get(key, 0) >= val:
                        continue
                    waited_dma[E][key] = val
                    op["waits"].append(("dma", key, val * 16))
                else:
                    Dn = dop["eng"]
                    if Dn == E:
                        if E == "pe" or typ != "raw":
                            continue
                    if waited_seq[E][Dn] >= dop["seq"]:
                        continue
                    waited_seq[E][Dn] = dop["seq"]
                    dop["signal"] = True
                    op["waits"].append(("cmp", d))
        sig_idx = {}
        nsig = {e: 0 for e in self.ENGS}
        for e in self.ENGS:
            for i in self.eng_ops[e]:
                if ops[i]["signal"]:
                    sig_idx[i] = nsig[e]
                    nsig[e] += 1
        stack = contextlib.ExitStack()
        sems = {}
        for e in self.ENGS:
            for g in range((nsig[e] + SIG_R - 1) // SIG_R):
                sems[("cmp", e, g)] = stack.enter_context(nc.semaphore(f"s_{e}_{g}"))
        for name, cnt in self.dma_cnt.items():
            for g in range((cnt + DMA_R - 1) // DMA_R):
                sems[("dma", name, g)] = stack.enter_context(nc.semaphore(f"d_{name}_{g}"))
        self.n_sems = len(sems)
        engmap = {"pe": "tensor", "act": "scalar", "dve": "vector", "pool": "gpsimd", "sp": "sync"}

        def emit_engine(e, eng):
            for i in self.eng_ops[e]:
                op = ops[i]
                for w in op["waits"]:
                    if w[0] == "dma":
                        _, key, val = w
                        eng.wait_ge(sems[("dma", key[0], key[1])], val)
                    else:
                        d = w[1]
                        si = sig_idx[d]
                        eng.wait_ge(sems[("cmp", ops[d]["eng"], si // SIG_R)], si % SIG_R + 1)
                if op["fn"] is None:
                    continue
                inst = op["fn"](eng)
                if inst is None:
                    continue
                if op["dma"] is not None:
                    inst.then_inc(sems[("dma", op["dma"], op["dcount"] // DMA_R)], 16)
                elif op["signal"]:
                    si = sig_idx[i]
                    inst.then_inc(sems[("cmp", e, si // SIG_R)], 1)

        with stack:
            with nc.Block() as block:
                for e in self.ENGS:
                    if not self.eng_ops[e]:
                        continue

                    def _mk(e=e):
                        def _f(eng):
                            emit_engine(e, eng)
                        return _f
                    getattr(block, engmap[e])(_mk())


def _consts():
    c = {}
    c["ident_f"] = np.eye(128, dtype=np.float32)
    c["ident_b"] = np.eye(128, dtype=np.float32).astype(ml_dtypes.bfloat16)
    j = np.arange(128)[:, None]
    i = np.arange(128)[None, :]
    tri = np.zeros((128, 4, 128), np.float32)
    tri[:, 0, :] = (j <= i) * (-1.0 / 16)
    tri[:, 1, :] = (j >= i) * (-1.0 / 16)
    tri[:, 2, :] = (j > i) * (-1.0 / 16)
    tri[:, 3, :] = (j < i) * (-1.0 / 16)
    c["tri"] = tri
    msk = np.zeros((128, 2, 128), np.float32)
    msk[:, 0, :] = (j <= i)
    msk[:, 1, :] = (j >= i)
    c["msk"] = msk
    pm = np.zeros((128, 4, 128), np.float32)
    for gi, w in enumerate((2, 4, 8, 16)):
        for blk in range(2):
            for pos in range(64):
                lo = min(max(pos - w // 2, 0), 64)
                hi = min(max(pos - w // 2 + w, 0), 64)
                cnt = hi - lo
                for jj in range(lo, hi):
                    pm[blk * 64 + jj, gi, blk * 64 + pos] += 1.0 / cnt
                pm[blk * 64 + pos, gi, blk * 64 + pos] -= 1.0
    c["poolm"] = pm.astype(ml_dtypes.bfloat16)
    c["ones_b"] = np.ones((1, 128), np.float32).astype(ml_dtypes.bfloat16)
    return c


DBG_OFFS = {}


class Arena:
    def __init__(self, ten, nbytes):
        self.ten = ten
        self.nbytes = nbytes
        self.off = 0
        self.gen = 0

    def reset(self):
        self.off = 0
        self.gen += 1

    def alloc(self, parts, nelem, dt, name=None):
        if name:
            DBG_OFFS[name] = (self.off, nelem, dt == F32)
        esz = 4 if dt == F32 else 2
        nb = (nelem * esz + 63) // 64 * 64
        assert self.off + nb <= self.nbytes, (self.off, nb, self.nbytes)
        a = self.ten[0:parts, self.off // 2:(self.off + nelem * esz) // 2]
        self.off += nb
        if dt == F32:
            a = a.bitcast(F32)
        return a


def build_program(layers=(0, 1), do_final=True, nseq=SEQ_PER_CORE, stop_after=None):
    nc = bass.Bass("TRN2", target_bir_lowering=False)

    def din(name, shape, dt=F32):
        return nc.dram_tensor(name, list(shape), dt, kind="ExternalInput").ap()

    def dint(name, shape, dt):
        return nc.dram_tensor(name, list(shape), dt, kind="Internal").ap()

    x4 = din("x4", [nseq, T, D])
    ctx4 = din("ctx4", [nseq, CTX, D])
    ccT = din("ccT", [D, 5])
    norm1_g = din("norm1_g", [2, D])
    norm2_g = din("norm2_g", [2, D])
    w_mod = din("w_mod", [2, D, 6 * D])
    b_mod = din("b_mod", [2, 6 * D])
    w_in = din("gla_w_in", [D, 3072])
    w_ga = din("gla_w_gate_a", [2, D, 16])
    w_gb = din("gla_w_gate_b", [2, 16, 512])
    b_g = din("gla_b_gate", [2, 512])
    gnorm = din("gla_norm_g", [1, 256])
    w_out = din("gla_w_out", [D, D])
    pool_w = din("pool_w", [4, 256, 256])
    pool_b = din("pool_b", [1, D])
    pool_scale = din("pool_scale", [1, D])
    w_router = din("w_router", [D, NE])
    b_router = din("b_router", [1, NE])
    w_gate_e = din("w_gate_e", [2, NE, D, DE])
    w_up_e = din("w_up_e", [2, NE, D, DE])
    w_down_e = din("w_down_e", [2, NE, DE, D])
    final_g = din("final_g", [1, D])
    ident_f = din("ident_f", [128, 128])
    ident_b = din("ident_b", [128, 128], BF16)
    tri_d = din("tri", [128, 4, 128])
    msk_d = din("msk", [128, 2, 128])
    poolm_d = din("poolm", [128, 4, 128], BF16)
    ones_d = din("ones_b", [1, 128], BF16)
    out4 = nc.dram_tensor("out4", [nseq, T, D], F32, kind="ExternalOutput").ap()

    WG = dint("WGs", [2, NE, D, DE], BF16)
    WU = dint("WUs", [2, NE, D, DE], BF16)
    WD = dint("WDs", [2, NE, DE, D], BF16)
    WIN = dint("WINs", [D, 3072], BF16)
    WOUT = dint("WOUTs", [D, D], BF16)
    WPOOL = dint("WPOOLs", [4, 256, 256], BF16)
    WGAs = dint("WGAs", [D, 32], BF16)
    MODS = dint("MODS", [2, 5, 6 * D], F32)

    st = contextlib.ExitStack()

    def sb(name, shape, dt):
        return st.enter_context(nc.sbuf_tensor(name, list(shape), dt))

    X = sb("X", [128, 16, D], F32)
    HT = sb("HT", [128, 8, T], BF16)
    ARENA_BYTES = 70 * 1024
    ARENA_T = sb("ARENA", [128, ARENA_BYTES // 2], BF16)
    ar = Arena(ARENA_T, ARENA_BYTES)
    BC = [sb(f"BC{i}", [128, D], F32) for i in range(3)]
    IDF = sb("IDF", [128, 128], F32)
    IDB = sb("IDB", [128, 128], BF16)
    TRI = sb("TRI", [128, 4, 128], F32)
    MSK = sb("MSK", [128, 2, 128], F32)
    POOLM = sb("POOLM", [128, 4, 128], BF16)
    ONES = sb("ONES", [1, 128], BF16)
    PBR = sb("PBR", [1, D], BF16)
    WR = sb("WR", [128, 8, NE], F32)
    BRB = sb("BRB", [128, 16, NE], F32)
    WGA = sb("WGA", [128, 8, 32], BF16)
    WGB = sb("WGB", [64, D], BF16)
    NGB = sb("NGB", [128, 256], F32)
    WP = sb("WP", [128, 4, 2, 256], BF16)
    SSQ = sb("SSQ", [128, 18], F32)
    SQT = sb("SQT", [128, 18], F32)
    RSTD = sb("RSTD", [128, 18], F32)
    SC = sb("SC", [128, 16, NE], F32)
    GATES = sb("GATES", [128, 16, NE], F32)
    RT = [sb(f"RT{i}", [128, 16, NE], F32) for i in range(4)]
    RS = [sb(f"RS{i}", [128, 64], F32) for i in range(4)]
    PS = [st.enter_context(nc.psum_tensor(f"PS{i}", [128, 512], F32)) for i in range(8)]

    p = Prog(nc)
    uid = [0]

    def U():
        uid[0] += 1
        return uid[0]

    rot = {}

    def dma(eng, out, in_, reads, writes, name, nrot=1):
        if nrot > 1:
            k = rot.get(name, 0)
            rot[name] = k + 1
            name = f"{name}_{k % nrot}"
        p.add(eng, lambda e: e.dma_start(out=out, in_=in_), reads=reads, writes=writes, dma=name)

    def pk(i, lo=0, hi=512):
        return [("PS", i, c) for c in range(lo // 128, (hi + 127) // 128)]

    def mm_group(out, pairs, reads, writes, f32=False):
        n = len(pairs)

        def fn(e):
            inst = None
            for q, (l, r) in enumerate(pairs):
                inst = e.matmul(out, lhsT=l, rhs=r, start=(q == 0), stop=(q == n - 1))
            return inst
        p.add("pe", fn, reads=reads, writes=writes)

    dma("sp", IDF[:], ident_f, [], ["IDF"], "c0", 8)
    dma("sp", IDB[:], ident_b, [], ["IDB"], "c0", 8)
    dma("sp", TRI[:], tri_d, [], ["TRI"], "c0", 8)
    dma("sp", MSK[:], msk_d, [], ["MSK"], "c0", 8)
    dma("sp", POOLM[:], poolm_d, [], ["POOLM"], "c0", 8)
    dma("sp", ONES[:], ones_d, [], ["ONES"], "c0", 8)
    dma("sp", WR[:], w_router.rearrange("(kc p) n -> p kc n", p=128), [], ["WR"], "c0", 8)
    dma("sp", NGB[:], gnorm.partition_broadcast(128), [], ["NGB"], "c0", 8)
    dma("sp", BRB[:, 0, :], b_router.partition_broadcast(128), [], ["BRB0"], "c0", 8)
    for b in range(1, 16):
        p.add("dve", lambda e, b=b: e.tensor_copy(out=BRB[:, b, :], in_=BRB[:, 0, :]), reads=["BRB0"], writes=[("BRB", b)])
    brb_keys = ["BRB0"] + [("BRB", b) for b in range(1, 16)]
    if 0 in layers:
        dma("pool", WIN, w_in, [], ["WIN"], "cast", 8)
        dma("pool", WOUT, w_out, [], ["WOUT"], "cast", 8)
        for z in range(2):
            dma("pool", WGAs[:, z * 16:(z + 1) * 16], w_ga[z], [], [("WGAs", z)], "cast", 8)
        p.add("pool", lambda e: e.memset(WGB[:], 0.0), writes=["WGB"])
        for z in range(2):
            dma("pool", WGB[z * 16:(z + 1) * 16, z * 512:(z + 1) * 512], w_gb[z], ["WGB"], [("WGBp", z)], "cast", 8)
            dma("pool", WGB[32:33, z * 512:(z + 1) * 512], b_g[z:z + 1, :], ["WGB"], [("WGBb", z)], "cast", 8)
        wgb_keys = ["WGB"] + [("WGBp", z) for z in range(2)] + [("WGBb", z) for z in range(2)]
        dma("sp", WGA[:], WGAs.rearrange("(kc p) n -> p kc n", p=128), [("WGAs", 0), ("WGAs", 1)], ["WGA"], "c0", 8)
    if 1 in layers:
        dma("pool", WPOOL, pool_w, [], ["WPOOL"], "cast", 8)
        dma("pool", PBR[:], pool_b, [], ["PBR"], "cast", 8)
        dma("sp", WP[:], WPOOL.rearrange("g (kc p) n -> p g kc n", p=128), ["WPOOL"], ["WP"], "c0", 8)
    for li in layers:
        if os.environ.get("K_DBG_NOEXP") == "1":
            break
        for e_ in range(NE):
            dma("pool", WG[li, e_], w_gate_e[li, e_], [], [("WG", li, e_)], "cast", 8)
            dma("pool", WU[li, e_], w_up_e[li, e_], [], [("WU", li, e_)], "cast", 8)
            dma("pool", WD[li, e_], w_down_e[li, e_], [], [("WD", li, e_)], "cast", 8)

    ar.reset()
    CC = ar.alloc(128, 8 * 5, F32).rearrange("p (a b) -> p a b", a=8)
    SCC = ar.alloc(128, 8 * 5, F32).rearrange("p (a b) -> p a b", a=8)
    WM = [ar.alloc(128, 8 * 512, F32).rearrange("p (a b) -> p a b", a=8) for _ in range(2)]
    BMT = [ar.alloc(5, 512, F32) for _ in range(2)]
    NGT = [ar.alloc(5, 512, F32) for _ in range(2)]
    MT = [ar.alloc(5, 512, F32) for _ in range(2)]
    dma("sp", CC, ccT.rearrange("(kc p) n -> p kc n", p=128), [], ["CC"], "c0", 8)
    p.add("act", lambda e: e.activation(out=SCC, in_=CC, func=AF.Silu), reads=["CC"], writes=["SCC"])
    q = 0
    for li in layers:
        for ct in range(12):
            b = q % 2
            q += 1
            dma("sp", WM[b], w_mod[li][:, ct * 512:(ct + 1) * 512].rearrange("(kc p) n -> p kc n", p=128), [], [("WM", b)], f"wm{b}", 3)
            dma("sp", BMT[b], b_mod[li:li + 1, ct * 512:(ct + 1) * 512].partition_broadcast(5), [], [("BMT", b)], f"wm{b}", 3)
            is_sc = ct in (2, 3, 8, 9)
            if is_sc:
                ng = norm1_g if ct < 6 else norm2_g
                dma("sp", NGT[b], ng[li:li + 1, (ct % 2) * 512:(ct % 2 + 1) * 512].partition_broadcast(5), [], [("NGT", b)], f"wm{b}", 3)
            mm_group(PS[b][0:5, :], [(SCC[:, kc, :], WM[b][:, kc, :]) for kc in range(8)],
                     reads=["SCC", ("WM", b)], writes=pk(b))
            p.add("dve", lambda e, b=b: e.tensor_tensor(out=MT[b], in0=PS[b][0:5, :], in1=BMT[b], op=ALU.add),
                  reads=pk(b) + [("BMT", b)], writes=[("MT", b)])
            if is_sc:
                p.add("dve", lambda e, b=b: e.scalar_tensor_tensor(out=MT[b], in0=MT[b], scalar=1.0, in1=NGT[b], op0=ALU.add, op1=ALU.mult),
                      reads=[("MT", b), ("NGT", b)], writes=[("MT", b)])
            dma("sp", MODS[li, :, ct * 512:(ct + 1) * 512], MT[b], [("MT", b)], [("MODS", li, ct)], f"mt{b}")

    def mods_row(li, row, part):
        return MODS[li, row:row + 1, part * D:(part + 1) * D].partition_broadcast(128)

    def mods_keys(li, part):
        return [("MODS", li, 2 * part), ("MODS", li, 2 * part + 1)]

    def load_bc(i, src, reads):
        dma("sp", BC[i][:], src, reads, [("BC", i)], f"bc{i}")

    def rms_stats(srcs, scale):
        n = len(srcs)
        junk = ar.alloc(128, D, F32)
        ju = U()
        for i, (a, rk) in enumerate(srcs):
            w = a.shape[-1]
            p.add("act", lambda e, a=a, i=i, w=w: e.activation(out=junk[:, 0:w], in_=a, func=AF.Square, accum_out=SSQ[:, i:i + 1]),
                  reads=rk, writes=[("junk", ju), ("SSQ", i)])
        p.add("act", lambda e: e.activation(out=SQT[:, 0:n], in_=SSQ[:, 0:n], func=AF.Sqrt, bias=EPS, scale=scale),
              reads=[("SSQ", i) for i in range(n)], writes=["SQT"])
        p.add("dve", lambda e: e.reciprocal(out=RSTD[:, 0:n], in_=SQT[:, 0:n]), reads=["SQT"], writes=["RSTD"])

    def xk(blk, half=None):
        if half is None:
            return [("X", blk, 0), ("X", blk, 1)]
        return [("X", blk, half)]

    for s in range(nseq):
        if stop_after == "pro":
            break
        for g4 in range(4):
            dma("sp", X[:, g4 * 4:(g4 + 1) * 4, :], x4[s, g4 * 512:(g4 + 1) * 512, :].rearrange("(b p) d -> p b d", p=128),
                [], [k for b in range(g4 * 4, g4 * 4 + 4) for k in xk(b)], "x", 4)
        for li in layers:
            p.barrier()
            ar.reset()
            if li == 0:
                HCT = ar.alloc(128, 8 * CTX, BF16, name='HCT').rearrange("p (a b) -> p a b", a=8)
                mk = ar.off
                CX = ar.alloc(128, 2 * D, F32, name='CX').rearrange("p (a b) -> p a b", a=2)
                T1 = [ar.alloc(128, D, F32, name=f'T1_{_i}') for _i in range(2)]
                HB = [ar.alloc(128, D, BF16, name=f'HB_{_i}') for _i in range(2)]
                dma("sp", CX, ctx4[s].rearrange("(b p) d -> p b d", p=128), [], ["CX"], "cx")
                load_bc(0, mods_row(0, s, 1), mods_keys(0, 1))
                load_bc(1, mods_row(0, s, 0), mods_keys(0, 0))
                rms_stats([(X[:, b, :], xk(b)) for b in range(16)] + [(CX[:, b, :], ["CX"]) for b in range(2)], 1.0 / D)
                for blk in range(18):
                    if blk == 16:
                        load_bc(0, mods_row(0, 4, 1), mods_keys(0, 1))
                        load_bc(1, mods_row(0, 4, 0), mods_keys(0, 0))
                    b2 = blk % 2
                    src = X[:, blk, :] if blk < 16 else CX[:, blk - 16, :]
                    srck = xk(blk) if blk < 16 else ["CX"]
                    p.add("dve", lambda e, src=src, b2=b2, blk=blk: e.scalar_tensor_tensor(
                        out=T1[b2], in0=src, scalar=RSTD[:, blk:blk + 1], in1=BC[0][:], op0=ALU.mult, op1=ALU.mult),
                        reads=srck + ["RSTD", ("BC", 0)], writes=[("T1", b2)])
                    p.add("pool", lambda e, b2=b2: e.tensor_tensor(out=HB[b2], in0=T1[b2], in1=BC[1][:], op=ALU.add),
                          reads=[("T1", b2), ("BC", 1)], writes=[("HB", b2)])
                    for hh in range(2):
                        pb = b2 * 2 + hh
                        for kc4 in range(4):
                            kc = hh * 4 + kc4
                            mm_group(PS[pb][:, kc4 * 128:(kc4 + 1) * 128], [(HB[b2][:, kc * 128:(kc + 1) * 128], IDB[:])],
                                     reads=[("HB", b2), "IDB"], writes=pk(pb, kc4 * 128, kc4 * 128 + 128))
                        if blk < 16:
                            dst = HT[:, hh * 4:(hh + 1) * 4, blk * 128:(blk + 1) * 128]
                            dk = [("HT", blk)]
                        else:
                            dst = HCT[:, hh * 4:(hh + 1) * 4, (blk - 16) * 128:(blk - 15) * 128]
                            dk = [("HCT", blk - 16)]
                        p.add("act", lambda e, dst=dst, pb=pb: e.activation(out=dst, in_=PS[pb][:].rearrange("p (a b) -> p a b", a=4), func=AF.Copy),
                              reads=pk(pb), writes=dk)
                if stop_after == "n1":
                    break
                p.barrier()
                ar.off = mk
                load_bc(2, mods_row(0, s, 2), mods_keys(0, 2))
                LOWT = ar.alloc(64, 2304, BF16)
                WINH = ar.alloc(128, 8 * 768, BF16).rearrange("p (a b) -> p a b", a=8)
                WOUTH = ar.alloc(128, 2 * D, BF16).rearrange("p (a b) -> p a b", a=2)
                OBUF = ar.alloc(128, 16 * 256, F32).rearrange("p (a b) -> p a b", a=16)
                E1 = [ar.alloc(128, 128, F32) for _ in range(2)]
                SP_ = [ar.alloc(128, 128, F32, name=f'SP{_i}') for _i in range(2)]
                EQR = [ar.alloc(128, 256, F32, name=f'EQR{_i}') for _i in range(3)]
                EK = [ar.alloc(128, 128, F32) for _ in range(2)]
                QT = [ar.alloc(128, 128, BF16) for _ in range(3)]
                KT = [ar.alloc(128, 128, BF16) for _ in range(3)]
                KH = [ar.alloc(128, 128, BF16) for _ in range(3)]
                VB = [ar.alloc(128, 256, BF16) for _ in range(3)]
                ATM = [ar.alloc(128, 128, BF16) for _ in range(2)]
                SS = [ar.alloc(128, 256, F32, name=f'SS{_i}') for _i in range(2)]
                SBF = [[ar.alloc(128, 256, BF16) for _ in range(2)] for _ in range(2)]
                SR = [ar.alloc(128, 256, F32) for _ in range(2)]
                TT = [ar.alloc(128, 256, F32) for _ in range(2)]
                OGB = [ar.alloc(128, 256, BF16) for _ in range(2)]
                OGT2 = [ar.alloc(128, 256, BF16).rearrange("p (a b) -> p a b", a=2) for _ in range(2)]
                TMP = [ar.alloc(128, 512, F32) for _ in range(4)]

                def hT(c, kc):
                    if c < 16:
                        return HT[:, kc, c * 128:(c + 1) * 128], ("HT", c)
                    return HCT[:, kc, (c - 16) * 128:(c - 15) * 128], ("HCT", c - 16)

                p.add("pool", lambda e: e.memset(LOWT, 1.0), writes=["LOWTm"])
                for tt in range(5):
                    if tt < 4:
                        rh = [HT[:, kc, tt * 512:(tt + 1) * 512] for kc in range(8)]
                        rk = [("HT", b) for b in range(tt * 4, tt * 4 + 4)]
                        w = 512
                    else:
                        rh = [HCT[:, kc, :] for kc in range(8)]
                        rk = [("HCT", 0), ("HCT", 1)]
                        w = 256
                    pb = tt % 2
                    mm_group(PS[pb][0:32, 0:w], [(WGA[:, kc, :], rh[kc]) for kc in range(8)], reads=rk + ["WGA"], writes=pk(pb, 0, w))
                    p.add("act", lambda e, tt=tt, w=w, pb=pb: e.activation(out=LOWT[0:32, tt * 512:tt * 512 + w], in_=PS[pb][0:32, 0:w], func=AF.Copy),
                          reads=pk(pb, 0, w) + ["LOWTm"], writes=[("LOWT", tt)])

                if stop_after == "low":
                    break
                stopped = False
                for h in range(4):
                    if stop_after == ("head", h):
                        stopped = True
                        break
                    hu = U()
                    dma("sp", WINH[:, :, 0:128], WIN[:, h * 128:(h + 1) * 128].rearrange("(kc p) n -> p kc n", p=128), ["WIN"], [("WINH", 0)], "winh", 5)
                    dma("sp", WINH[:, :, 128:256], WIN[:, 512 + h * 128:512 + (h + 1) * 128].rearrange("(kc p) n -> p kc n", p=128), ["WIN"], [("WINH", 1)], "winh", 5)
                    dma("sp", WINH[:, :, 256:512], WIN[:, 1024 + h * 256:1024 + (h + 1) * 256].rearrange("(kc p) n -> p kc n", p=128), ["WIN"], [("WINH", 2)], "winh", 5)
                    dma("sp", WINH[:, :, 512:768], WIN[:, 2048 + h * 256:2048 + (h + 1) * 256].rearrange("(kc p) n -> p kc n", p=128), ["WIN"], [("WINH", 3)], "winh", 5)
                    dma("sp", WOUTH, WOUT[h * 256:(h + 1) * 256, :].rearrange("(kc p) n -> p kc n", p=128), ["WOUT"], ["WOUTH"], "winh", 5)
                    for dr in range(2):
                        p.add("pool", lambda e, dr=dr: e.memset(SS[dr], 0.0), writes=[("SS", dr)])
                        p.add("pool", lambda e, dr=dr: e.memset(SBF[dr][0], 0.0), writes=[("SBF", dr, 0)])
                    order = [[16, 17] + list(range(16)), [17, 16] + list(range(15, -1, -1))]
                    steps = []
                    for i in range(18):
                        steps.append((0, order[0][i], i))
                        steps.append((1, order[1][i], i))

                    def S1(k, dr, c, i):
                        r2 = k % 2
                        A0, A1, ZB = 3 * r2, 3 * r2 + 1, 3 * r2 + 2
                        hts = [hT(c, kc) for kc in range(8)]
                        hk = [hts[0][1]]
                        zc = dr * 512 + h * 128
                        mm_group(PS[ZB][:, 0:128], [(LOWT[0:64, c * 128:(c + 1) * 128], WGB[0:64, zc:zc + 128])],
                                 reads=[("LOWT", c // 4), "LOWTm"] + wgb_keys, writes=pk(ZB))
                        p.add("act", lambda e: e.activation(out=E1[r2], in_=PS[ZB][:, 0:128], func=AF.Exp, scale=-1.0),
                              reads=pk(ZB), writes=[("E1", r2)])
                        p.add("act", lambda e: e.activation(out=SP_[r2], in_=E1[r2], func=AF.Ln, bias=1.0),
                              reads=[("E1", r2)], writes=[("SP", r2)])
                        mm_group(PS[A0][:, 0:384], [(hts[kc][0], WINH[:, kc, 128:512]) for kc in range(8)],
                                 reads=hk + [("WINH", 1), ("WINH", 2)], writes=pk(A0))
                        mm_group(PS[A0][:, 384:512], [(WINH[:, kc, 128:256], hts[kc][0]) for kc in range(8)],
                                 reads=hk + [("WINH", 1)], writes=pk(A0))
                        mm_group(PS[A1][:, 0:128], [(WINH[:, kc, 0:128], hts[kc][0]) for kc in range(8)],
                                 reads=hk + [("WINH", 0)], writes=pk(A1))

                    def S2(k, dr, c, i):
                        r2 = k % 2
                        r3 = k % 3
                        A0, A1, ZB = 3 * r2, 3 * r2 + 1, 3 * r2 + 2
                        mm_group(PS[ZB][:, 128:256], [(SP_[r2], TRI[:, dr, :])], reads=[("SP", r2), "TRI"], writes=pk(ZB))
                        mm_group(PS[ZB][:, 256:384], [(TRI[:, 2 + dr, :], SP_[r2])], reads=[("SP", r2), "TRI"], writes=pk(ZB))
                        p.add("act", lambda e: e.activation(out=EQR[r3], in_=PS[ZB][:, 128:384], func=AF.Exp),
                              reads=pk(ZB), writes=[("EQR", r3)])
                        p.add("act", lambda e: e.activation(out=EK[r2], in_=PS[ZB][:, 128:256], func=AF.Exp, scale=-1.0),
                              reads=pk(ZB), writes=[("EK", r2)])
                        p.add("act", lambda e: e.activation(out=VB[r3], in_=PS[A0][:, 128:384], func=AF.Copy),
                              reads=pk(A0), writes=[("VB", r3)])
                        p.add("dve", lambda e: e.scalar_tensor_tensor(out=QT[r3], in0=PS[A1][:, 0:128], scalar=float(128 ** -0.5), in1=EQR[r3][:, 0:128],
                                                                      op0=ALU.mult, op1=ALU.mult),
                              reads=pk(A1) + [("EQR", r3)], writes=[("QT", r3)])
                        p.add("dve", lambda e: e.tensor_tensor(out=KT[r3], in0=PS[A0][:, 384:512], in1=EK[r2], op=ALU.mult),
                              reads=pk(A0) + [("EK", r2)], writes=[("KT", r3)])
                        p.add("dve", lambda e: e.tensor_tensor(out=KH[r3], in0=PS[A0][:, 0:128], in1=EQR[r3][:, 128:256], op=ALU.mult),
                              reads=pk(A0) + [("EQR", r3)], writes=[("KH", r3)])

                    def S3(k, dr, c, i):
                        r2 = k % 2
                        r3 = k % 3
                        A1 = 3 * r2 + 1
                        mm_group(PS[A1][:, 128:256], [(KT[r3], QT[r3])], reads=[("KT", r3), ("QT", r3)], writes=pk(A1))
                        p.add("dve", lambda e: e.tensor_tensor(out=ATM[r2], in0=PS[A1][:, 128:256], in1=MSK[:, dr, :], op=ALU.mult),
                              reads=pk(A1) + ["MSK"], writes=[("ATM", r2)])

                    def S4(k, dr, c, i):
                        r2 = k % 2
                        r3 = k % 3
                        BO = 6 + dr
                        sb_cur = SBF[dr][i % 2]
                        sb_nxt = SBF[dr][(i + 1) % 2]
                        if c < 16:
                            mm_group(PS[BO][:, 0:256], [(ATM[r2], VB[r3]), (QT[r3], sb_cur)],
                                     reads=[("ATM", r2), ("VB", r3), ("QT", r3), ("SBF", dr, i % 2)], writes=pk(BO))
                        mm_group(PS[BO][:, 256:512], [(KH[r3], VB[r3])], reads=[("KH", r3), ("VB", r3)], writes=pk(BO))
                        if c < 16:
                            first = (dr == 0) if c <= 7 else (dr == 1)
                            if first:
                                p.add("act", lambda e: e.activation(out=OBUF[:, c, :], in_=PS[BO][:, 0:256], func=AF.Copy),
                                      reads=pk(BO), writes=[("OBUF", c)])
                            else:
                                p.add("dve", lambda e: e.tensor_tensor(out=OBUF[:, c, :], in0=PS[BO][:, 0:256], in1=OBUF[:, c, :], op=ALU.add),
                                      reads=pk(BO) + [("OBUF", c)], writes=[("OBUF", c)])
                        edge = 127 if dr == 0 else 0
                        p.add("dve", lambda e: e.scalar_tensor_tensor(out=SS[dr], in0=SS[dr], scalar=EQR[r3][:, edge:edge + 1], in1=PS[BO][:, 256:512],
                                                                      op0=ALU.mult, op1=ALU.add),
                              reads=[("SS", dr), ("EQR", r3)] + pk(BO), writes=[("SS", dr)])
                        p.add("pool", lambda e: e.tensor_copy(out=sb_nxt, in_=SS[dr]), reads=[("SS", dr)], writes=[("SBF", dr, (i + 1) % 2)])

                    nst = len(steps)
                    for it in range(nst + 3):
                        if it < nst:
                            S1(it, *steps[it])
                        if 0 <= it - 1 < nst:
                            S2(it - 1, *steps[it - 1])
                        if 0 <= it - 2 < nst:
                            S3(it - 2, *steps[it - 2])
                        if 0 <= it - 3 < nst:
                            S4(it - 3, *steps[it - 3])

                    if stop_after == ("scan", h):
                        stopped = True
                        break
                    junk = TT[0]
                    for c in range(16):
                        p.add("act", lambda e, c=c: e.activation(out=junk, in_=OBUF[:, c, :], func=AF.Square, accum_out=SSQ[:, c:c + 1]),
                              reads=[("OBUF", c)], writes=[("TT", 0), ("SSQ", c)])
                    p.add("act", lambda e: e.activation(out=SQT[:, 0:16], in_=SSQ[:, 0:16], func=AF.Sqrt, bias=EPS, scale=1.0 / 256),
                          reads=[("SSQ", i) for i in range(16)], writes=["SQT"])
                    p.add("dve", lambda e: e.reciprocal(out=RSTD[:, 0:16], in_=SQT[:, 0:16]), reads=["SQT"], writes=["RSTD"])
                    def ro1(c):
                        r = c % 2
                        b0 = 4 * r
                        hts = [hT(c, kc) for kc in range(8)]
                        mm_group(PS[b0][:, 0:256], [(hts[kc][0], WINH[:, kc, 512:768]) for kc in range(8)],
                                 reads=[hts[0][1], ("WINH", 3)], writes=pk(b0, 0, 256))
                        p.add("act", lambda e: e.activation(out=SR[r], in_=PS[b0][:, 0:256], func=AF.Silu),
                              reads=pk(b0, 0, 256), writes=[("SR", r)])
                        p.add("dve", lambda e: e.scalar_tensor_tensor(out=TT[r], in0=OBUF[:, c, :], scalar=RSTD[:, c:c + 1], in1=NGB[:],
                                                                      op0=ALU.mult, op1=ALU.mult),
                              reads=[("OBUF", c), "RSTD", "NGB"], writes=[("TT", r)])
                        p.add("pool", lambda e: e.tensor_tensor(out=OGB[r], in0=TT[r], in1=SR[r], op=ALU.mult),
                              reads=[("TT", r), ("SR", r)], writes=[("OGB", r)])

                    def ro2(c):
                        r = c % 2
                        b0 = 4 * r
                        for fc in range(2):
                            mm_group(PS[b0 + 3][:, fc * 128:(fc + 1) * 128], [(OGB[r][:, fc * 128:(fc + 1) * 128], IDB[:])],
                                     reads=[("OGB", r), "IDB"], writes=pk(b0 + 3))
                        p.add("act", lambda e: e.activation(out=OGT2[r], in_=PS[b0 + 3][:, 0:256].rearrange("p (a b) -> p a b", a=2), func=AF.Copy),
                              reads=pk(b0 + 3), writes=[("OGT2", r)])

                    def ro3(c):
                        r = c % 2
                        b0 = 4 * r
                        for half in range(2):
                            pb = b0 + 1 + half
                            ti = r * 2 + half
                            mm_group(PS[pb][:, :], [(OGT2[r][:, kc2, :], WOUTH[:, kc2, half * 512:(half + 1) * 512]) for kc2 in range(2)],
                                     reads=[("OGT2", r), "WOUTH"], writes=pk(pb))
                            p.add("dve", lambda e, pb=pb, ti=ti, half=half: e.tensor_tensor(out=TMP[ti], in0=PS[pb][:, :], in1=BC[2][:, half * 512:(half + 1) * 512], op=ALU.mult),
                                  reads=pk(pb) + [("BC", 2)], writes=[("TMP", ti)])
                            p.add("pool", lambda e, ti=ti, half=half: e.tensor_tensor(out=X[:, c, half * 512:(half + 1) * 512], in0=X[:, c, half * 512:(half + 1) * 512], in1=TMP[ti], op=ALU.add),
                                  reads=[("TMP", ti)] + xk(c, half), writes=xk(c, half))

                    for c in range(-1, 17):
                        if 0 <= c + 1 < 16:
                            ro1(c + 1)
                        if 0 <= c < 16:
                            ro2(c)
                        if 0 <= c - 1 < 16:
                            ro3(c - 1)
                if stopped:
                    break
            else:
                T1p = [ar.alloc(128, D, F32) for _ in range(2)]
                HBp = [ar.alloc(128, D, BF16) for _ in range(2)]
                PT = [ar.alloc(128, 8 * 128, BF16).rearrange("p (a b) -> p a b", a=8) for _ in range(2)]
                TMPY = [ar.alloc(128, D, F32) for _ in range(2)]
                load_bc(0, mods_row(1, s, 1), mods_keys(1, 1))
                load_bc(1, mods_row(1, s, 0), mods_keys(1, 0))
                load_bc(2, mods_row(1, s, 2), mods_keys(1, 2))
                dma("sp", TMPY[0], pool_scale.partition_broadcast(128), [], [("TMPY", 0)], "psc")
                p.add("dve", lambda e: e.tensor_tensor(out=BC[2][:], in0=BC[2][:], in1=TMPY[0], op=ALU.mult),
                      reads=[("BC", 2), ("TMPY", 0)], writes=[("BC", 2)])
                rms_stats([(X[:, b, :], xk(b)) for b in range(16)], 1.0 / D)
                for blk in range(16):
                    r = blk % 2
                    b0 = 4 * r
                    p.add("dve", lambda e, r=r, blk=blk: e.scalar_tensor_tensor(
                        out=T1p[r], in0=X[:, blk, :], scalar=RSTD[:, blk:blk + 1], in1=BC[0][:], op0=ALU.mult, op1=ALU.mult),
                        reads=xk(blk) + ["RSTD", ("BC", 0)], writes=[("T1", r)])
                    p.add("pool", lambda e, r=r: e.tensor_tensor(out=HBp[r], in0=T1p[r], in1=BC[1][:], op=ALU.add),
                          reads=[("T1", r), ("BC", 1)], writes=[("HB", r)])
                    for hh in range(2):
                        pb = b0 + hh
                        for kc4 in range(4):
                            fc = hh * 4 + kc4
                            mm_group(PS[pb][:, kc4 * 128:(kc4 + 1) * 128], [(HBp[r][:, fc * 128:(fc + 1) * 128], POOLM[:, fc // 2, :])],
                                     reads=[("HB", r), "POOLM"], writes=pk(pb, kc4 * 128, kc4 * 128 + 128))
                        p.add("act", lambda e, r=r, hh=hh, pb=pb: e.activation(out=PT[r][:, hh * 4:(hh + 1) * 4, :], in_=PS[pb][:].rearrange("p (a b) -> p a b", a=4), func=AF.Copy),
                              reads=pk(pb), writes=[("PT", r, hh)])
                    for g in range(4):
                        pb = b0 + 2 + g // 2
                        lo = (g % 2) * 256
                        mm_group(PS[pb][:, lo:lo + 256],
                                 [(PT[r][:, 2 * g, :], WP[:, g, 0, :]), (PT[r][:, 2 * g + 1, :], WP[:, g, 1, :]), (ONES[0:1, :], PBR[0:1, g * 256:(g + 1) * 256])],
                                 reads=[("PT", r, g // 2), "WP", "ONES", "PBR"], writes=pk(pb, lo, lo + 256))
                    for half in range(2):
                        pb = b0 + 2 + half
                        p.add("dve", lambda e, r=r, half=half, pb=pb: e.tensor_tensor(out=TMPY[r][:, half * 512:(half + 1) * 512], in0=PS[pb][:, :], in1=BC[2][:, half * 512:(half + 1) * 512], op=ALU.mult),
                              reads=pk(pb) + [("BC", 2)], writes=[("TMPY", r)])
                    p.add("pool", lambda e, r=r, blk=blk: e.tensor_tensor(out=X[:, blk, :], in0=X[:, blk, :], in1=TMPY[r], op=ALU.add),
                          reads=[("TMPY", r)] + xk(blk), writes=xk(blk))
            if stop_after == ("mix", li):
                break
            p.barrier()
            ar.reset()
            T1n = [ar.alloc(128, D, F32) for _ in range(2)]
            H2F = [ar.alloc(128, 8 * 128, F32).rearrange("p (a b) -> p a b", a=8) for _ in range(2)]
            load_bc(0, mods_row(li, s, 4), mods_keys(li, 4))
            load_bc(1, mods_row(li, s, 3), mods_keys(li, 3))
            rms_stats([(X[:, b, :], xk(b)) for b in range(16)], 1.0 / D)
            for blk in range(16):
                r = blk % 2
                b0 = 4 * r
                p.add("dve", lambda e, r=r, blk=blk: e.scalar_tensor_tensor(
                    out=T1n[r], in0=X[:, blk, :], scalar=RSTD[:, blk:blk + 1], in1=BC[0][:], op0=ALU.mult, op1=ALU.mult),
                    reads=xk(blk) + ["RSTD", ("BC", 0)], writes=[("T1", r)])
                p.add("pool", lambda e, r=r: e.tensor_tensor(out=T1n[r], in0=T1n[r], in1=BC[1][:], op=ALU.add),
                      reads=[("T1", r), ("BC", 1)], writes=[("T1", r)])
                for hh in range(2):
                    pb = b0 + hh
                    for kc4 in range(4):
                        kc = hh * 4 + kc4
                        mm_group(PS[pb][:, kc4 * 128:(kc4 + 1) * 128], [(T1n[r][:, kc * 128:(kc + 1) * 128], IDF[:])],
                                 reads=[("T1", r), "IDF"], writes=pk(pb, kc4 * 128, kc4 * 128 + 128))
                    p.add("act", lambda e, hh=hh, pb=pb, blk=blk: e.activation(out=HT[:, hh * 4:(hh + 1) * 4, blk * 128:(blk + 1) * 128],
                                                                                 in_=PS[pb][:].rearrange("p (a b) -> p a b", a=4), func=AF.Copy),
                          reads=pk(pb), writes=[("HT", blk)] if hh == 0 else [("HTb", blk)])
                    p.add("dve", lambda e, r=r, hh=hh, pb=pb: e.tensor_copy(out=H2F[r][:, hh * 4:(hh + 1) * 4, :], in_=PS[pb][:].rearrange("p (a b) -> p a b", a=4)),
                          reads=pk(pb), writes=[("H2F", r, hh)])
                mm_group(PS[b0 + 2][:, 0:NE], [(H2F[r][:, kc, :], WR[:, kc, :]) for kc in range(8)],
                         reads=[("H2F", r, 0), ("H2F", r, 1), "WR"], writes=pk(b0 + 2, 0, 128))
                p.add("act", lambda e, blk=blk, b0=b0: e.activation(out=SC[:, blk, :], in_=PS[b0 + 2][:, 0:NE], func=AF.Sigmoid),
                      reads=pk(b0 + 2, 0, 128), writes=[("SC", blk)])
            sck = [("SC", b) for b in range(16)]
            SEL, CH, WT, _r3 = RT
            THR, M1, GS, TMPR = RS
            GM = SQT[:, 0:16]
            DEN = SSQ[:, 0:16]

            def dv(fn, reads, writes):
                p.add("dve", fn, reads=reads, writes=writes)
            dv(lambda e: e.tensor_tensor(out=SEL[:], in0=SC[:], in1=BRB[:], op=ALU.add), sck + brb_keys, ["SEL"])
            selv = SEL[:].rearrange("p b (g j) -> p (b g) j", j=4)
            a_ = [selv[:, :, j] for j in range(4)]
            pairs = [(0, 1), (0, 2), (0, 3), (1, 2), (1, 3), (2, 3)]
            for qi, (i0, i1) in enumerate(pairs):
                if qi == 0:
                    dv(lambda e, i0=i0, i1=i1: e.tensor_tensor(out=THR[:], in0=a_[i0], in1=a_[i1], op=ALU.min), ["SEL"], ["THR"])
                else:
                    dv(lambda e, i0=i0, i1=i1: e.tensor_tensor(out=TMPR[:], in0=a_[i0], in1=a_[i1], op=ALU.min), ["SEL"], ["TMPR"])
                    dv(lambda e: e.tensor_tensor(out=THR[:], in0=THR[:], in1=TMPR[:], op=ALU.max), ["THR", "TMPR"], ["THR"])
            dv(lambda e: e.tensor_reduce(out=M1[:], in_=selv, axis=AX.X, op=ALU.max), ["SEL"], ["M1"])
            dv(lambda e: e.tensor_tensor(out=GS[:], in0=M1[:], in1=THR[:], op=ALU.add), ["M1", "THR"], ["GS"])
            gsv = GS[:].rearrange("p (b g) -> p b g", g=4)
            dv(lambda e: e.tensor_reduce(out=GM, in_=gsv, axis=AX.X, op=ALU.max), ["GS"], ["GM"])
            isg = M1[:].rearrange("p (b g) -> p b g", g=4)
            dv(lambda e: e.tensor_tensor(out=isg, in0=gsv, in1=GM.unsqueeze(2).broadcast_to([128, 16, 4]), op=ALU.is_equal), ["GS", "GM", "M1"], ["ISG"])
            chv = CH[:].rearrange("p b (g j) -> p (b g) j", j=4)
            dv(lambda e: e.tensor_tensor(out=chv, in0=selv, in1=THR[:].unsqueeze(2).broadcast_to([128, 64, 4]), op=ALU.is_ge), ["SEL", "THR"], ["CH"])
            dv(lambda e: e.tensor_tensor(out=chv, in0=chv, in1=M1[:].unsqueeze(2).broadcast_to([128, 64, 4]), op=ALU.mult), ["CH", "ISG"], ["CH"])
            dv(lambda e: e.tensor_tensor(out=WT[:], in0=SC[:], in1=CH[:], op=ALU.mult), sck + ["CH"], ["WT"])
            dv(lambda e: e.tensor_reduce(out=DEN, in_=WT[:], axis=AX.X, op=ALU.add), ["WT"], ["DEN"])
            dv(lambda e: e.reciprocal(out=GM, in_=DEN), ["DEN", "GM"], ["RDEN"])
            dv(lambda e: e.tensor_tensor(out=GATES[:], in0=WT[:], in1=GM.unsqueeze(2).broadcast_to([128, 16, NE]), op=ALU.mult), ["WT", "RDEN"], ["GATES"])

            p.barrier()
            ar.reset()
            W1 = [ar.alloc(128, 8 * DE, BF16).rearrange("p (a b) -> p a b", a=8) for _ in range(2)]
            W3 = [ar.alloc(128, 8 * DE, BF16).rearrange("p (a b) -> p a b", a=8) for _ in range(2)]
            W2 = [ar.alloc(128, 4 * D, BF16).rearrange("p (a b) -> p a b", a=4) for _ in range(2)]
            HE = [ar.alloc(128, 4 * 512, BF16).rearrange("p (a b) -> p a b", a=4) for _ in range(2)]
            SG = [ar.alloc(128, 512, BF16) for _ in range(2)]
            G2B = ar.alloc(128, D, BF16)
            load_bc(0, mods_row(li, s, 5), mods_keys(li, 5))
            p.add("act", lambda e: e.activation(out=G2B, in_=BC[0][:], func=AF.Copy), reads=[("BC", 0)], writes=["G2B"])
            def moe_gu(ex, t, hb):
                wb = ex % 2
                if t == 0:
                    dma("sp", W1[wb], WG[li, ex].rearrange("(kc p) n -> p kc n", p=128), [("WG", li, ex)], [("W1", wb)], f"w1{wb}")
                    dma("sp", W3[wb], WU[li, ex].rearrange("(kc p) n -> p kc n", p=128), [("WU", li, ex)], [("W3", wb)], f"w3{wb}")
                    dma("sp", W2[wb], WD[li, ex].rearrange("(kc p) n -> p kc n", p=128), [("WD", li, ex)], [("W2", wb)], f"w2{wb}")
                    p.add("pool", lambda e: e.tensor_tensor(out=W2[wb], in0=W2[wb], in1=G2B.unsqueeze(1).broadcast_to([128, 4, D]), op=ALU.mult),
                          reads=[("W2", wb), "G2B"], writes=[("W2", wb)])
                htk = [("HT", b_) for b_ in range(t * 4, t * 4 + 4)] + [("HTb", b_) for b_ in range(t * 4, t * 4 + 4)]
                for m in range(4):
                    pg = m % 2
                    pu = 2 + m % 2
                    mm_group(PS[pg][:, :], [(W1[wb][:, kc, m * 128:(m + 1) * 128], HT[:, kc, t * 512:(t + 1) * 512]) for kc in range(8)],
                             reads=htk + [("W1", wb)], writes=pk(pg))
                    mm_group(PS[pu][:, :], [(W3[wb][:, kc, m * 128:(m + 1) * 128], HT[:, kc, t * 512:(t + 1) * 512]) for kc in range(8)],
                             reads=htk + [("W3", wb)], writes=pk(pu))
                    p.add("act", lambda e, pg=pg: e.activation(out=SG[pg], in_=PS[pg][:, :], func=AF.Silu), reads=pk(pg), writes=[("SG", pg)])
                    p.add("dve", lambda e, pg=pg, pu=pu, m=m: e.tensor_tensor(out=HE[hb][:, m, :], in0=PS[pu][:, :], in1=SG[pg], op=ALU.mult),
                          reads=pk(pu) + [("SG", pg)], writes=[("HE", hb, m)])

            def moe_down(ex, t, hb):
                wb = ex % 2
                for tb in range(4):
                    blk = t * 4 + tb
                    for half in range(2):
                        pd = 4 + (tb * 2 + half) % 4
                        mm_group(PS[pd][:, :], [(HE[hb][:, mc, tb * 128:(tb + 1) * 128], W2[wb][:, mc, half * 512:(half + 1) * 512]) for mc in range(4)],
                                 reads=[("HE", hb, m) for m in range(4)] + [("W2", wb)], writes=pk(pd))
                        p.add("dve", lambda e, pd=pd, blk=blk, half=half: e.scalar_tensor_tensor(
                            out=X[:, blk, half * 512:(half + 1) * 512], in0=PS[pd][:, :], scalar=GATES[:, blk, ex:ex + 1],
                            in1=X[:, blk, half * 512:(half + 1) * 512], op0=ALU.mult, op1=ALU.add),
                            reads=pk(pd) + ["GATES"] + xk(blk, half), writes=xk(blk, half))

            iters = [(ex, t) for ex in range(NE) for t in range(4)]
            for q in range(len(iters) + 1):
                if q < len(iters):
                    moe_gu(iters[q][0], iters[q][1], q % 2)
                if q >= 1:
                    moe_down(iters[q - 1][0], iters[q - 1][1], (q - 1) % 2)
            if stop_after == ("layer", li):
                break
        p.barrier()
        ar.reset()
        T1f = [ar.alloc(128, D, F32) for _ in range(4)]
        if do_final and stop_after is None:
            load_bc(0, final_g.partition_broadcast(128), [])
            rms_stats([(X[:, b, :], xk(b)) for b in range(16)], 1.0 / D)
            for blk in range(16):
                r = blk % 4
                p.add("dve", lambda e, r=r, blk=blk: e.scalar_tensor_tensor(
                    out=T1f[r], in0=X[:, blk, :], scalar=RSTD[:, blk:blk + 1], in1=BC[0][:], op0=ALU.mult, op1=ALU.mult),
                    reads=xk(blk) + ["RSTD", ("BC", 0)], writes=[("T1", r)])
                dma("sp", out4[s, blk * 128:(blk + 1) * 128, :], T1f[r], [("T1", r)], [("OUT", s, blk)], f"o{r}")
        else:
            for g4 in range(4):
                dma("sp", out4[s, g4 * 512:(g4 + 1) * 512, :].rearrange("(b p) d -> p b d", p=128), X[:, g4 * 4:(g4 + 1) * 4, :],
                    [k for b in range(g4 * 4, g4 * 4 + 4) for k in xk(b)], [("OUT", s, g4)], "od", 4)
    p.barrier()
    p.finalize_and_emit()
    st.close()
    return nc, p


import os
DBG_A = int(os.environ.get('K_DBG_A', 9))
DBG_B = int(os.environ.get('K_DBG_B', 9))
_CACHE = {}


def _run(inputs, layers, do_final, nseq=SEQ_PER_CORE, stop_after=None, x_override=None, ncores=NCORES):
    key = (tuple(layers), do_final, nseq, stop_after)
    if key not in _CACHE:
        _CACHE[key] = build_program(layers, do_final, nseq, stop_after)
    nc, _ = _CACHE[key]
    cst = _consts()
    f = lambda a: np.ascontiguousarray(np.asarray(a, dtype=np.float32))
    x = f(inputs["x"]) if x_override is None else x_override
    c = f(inputs["c"])
    ctx = f(inputs["ctx"])
    c_ctx = f(inputs["c_ctx"])
    shared = dict(
        norm1_g=f(inputs["norm1_g"]), norm2_g=f(inputs["norm2_g"]), w_mod=f(inputs["w_mod"]), b_mod=f(inputs["b_mod"]),
        gla_w_in=f(inputs["gla_w_in"])[0], gla_w_gate_a=f(inputs["gla_w_gate_a"])[0], gla_w_gate_b=f(inputs["gla_w_gate_b"])[0],
        gla_b_gate=f(inputs["gla_b_gate"])[0], gla_norm_g=f(inputs["gla_norm_g"]), gla_w_out=f(inputs["gla_w_out"])[0],
        pool_w=f(inputs["pool_w"])[0], pool_b=f(inputs["pool_b"]).reshape(1, D), pool_scale=f(inputs["pool_scale"]).reshape(1, D),
        w_router=f(inputs["w_router"]), b_router=f(inputs["b_router"]).reshape(1, NE),
        w_gate_e=f(inputs["w_gate_e"]), w_up_e=f(inputs["w_up_e"]), w_down_e=f(inputs["w_down_e"]),
        final_g=f(inputs["final_g"]).reshape(1, D), **cst)
    in_maps = []
    for k in range(ncores):
        b0 = k * nseq
        m = dict(shared)
        m["x4"] = np.ascontiguousarray(x[b0:b0 + nseq])
        m["ctx4"] = np.ascontiguousarray(ctx[b0:b0 + nseq])
        cc = np.concatenate([c[b0:b0 + nseq], np.zeros((4 - nseq, D), np.float32), c_ctx[None, :]], axis=0)
        m["ccT"] = np.ascontiguousarray(cc.T)
        in_maps.append(m)
    res = run_bass_kernel_spmd(nc, in_maps, core_ids=list(range(ncores)))
    return np.concatenate([np.asarray(r["out4"]) for r in res.results], axis=0)


def kernel(**inputs):
    return _run(inputs, (0, 1), True).astype(np.float32)
```

```python
import contextlib
import os
import numpy as np
import ml_dtypes
import concourse.bass as bass
import concourse.mybir as mybir
from concourse.bass_utils import run_bass_kernel_spmd

F32 = mybir.dt.float32
BF16 = mybir.dt.bfloat16
AF = mybir.ActivationFunctionType
ALU = mybir.AluOpType
AX = mybir.AxisListType

SIG_R = 4000
DMA_R = 250
EPS = 1e-6
NCORES = 8
SEQ_PER_CORE = 4
T = 2048
D = 1024
CTX = 256
NE = 16
DE = 512


class Prog:
    ENGS = ("pe", "act", "dve", "pool", "sp")

    def __init__(self, nc):
        self.nc = nc
        self.ops = []
        self.keys = {}
        self.eng_ops = {e: [] for e in self.ENGS}
        self.dma_cnt = {}
        self.last_dma = {}

    def add(self, eng, fn, reads=(), writes=(), dma=None, extra_deps=None):
        idx = len(self.ops)
        deps = {}
        psk = {("PS", k[1]) for k in list(reads) + list(writes) if isinstance(k, tuple) and k[0] == "PS"}
        if psk:
            reads = [k for k in reads if not (isinstance(k, tuple) and k[0] == "PS")]
            writes = [k for k in writes if not (isinstance(k, tuple) and k[0] == "PS")] + sorted(psk)
        for k in reads:
            st = self.keys.get(k)
            if st is not None and st[0] is not None:
                deps.setdefault(st[0], "raw")
        for k in writes:
            st = self.keys.get(k)
            if st is not None:
                if st[0] is not None and st[0] not in deps:
                    deps[st[0]] = "waw"
                for r in st[1]:
                    if r not in deps:
                        deps[r] = "war"
        if extra_deps:
            for d in extra_deps:
                deps[d] = "raw"
        if dma is not None and dma in self.last_dma:
            deps[self.last_dma[dma]] = "raw"
        for k in reads:
            st = self.keys.setdefault(k, [None, []])
            if dma is None:
                st[1] = [r for r in st[1] if not (self.ops[r]["dma"] is None and self.ops[r]["eng"] == eng)]
            st[1].append(idx)
        for k in writes:
            self.keys[k] = [idx, []]
        dcount = None
        if dma is not None:
            dcount = self.dma_cnt.get(dma, 0)
            self.dma_cnt[dma] = dcount + 1
            self.last_dma[dma] = idx
        op = dict(eng=eng, fn=fn, deps=deps, dma=dma, dcount=dcount, seq=len(self.eng_ops[eng]),
                  waits=[], signal=False)
        self.ops.append(op)
        self.eng_ops[eng].append(idx)
        return idx

    def barrier(self):
        last = {}
        for e in self.ENGS:
            for i in reversed(self.eng_ops[e]):
                if self.ops[i]["dma"] is None and self.ops[i]["fn"] is not None:
                    last[e] = i
                    break
        dl = list(self.last_dma.values())
        for e in self.ENGS:
            deps = [i for (d, i) in last.items() if d != e] + dl
            self.add(e, None, extra_deps=deps)

    def finalize_and_emit(self):
        nc = self.nc
        ops = self.ops
        waited_seq = {e: {d: -1 for d in self.ENGS} for e in self.ENGS}
        waited_dma = {e: {} for e in self.ENGS}
        for i, op in enumerate(ops):
            E = op["eng"]
            for d, typ in op["deps"].items():
                dop = ops[d]
                if dop["dma"] is not None:
                    key = (dop["dma"], dop["dcount"] // DMA_R)
                    val = dop["dcount"] % DMA_R + 1
                    if waited_dma[E].get(key, 0) >= val:
                        continue
                    waited_dma[E][key] = val
                    op["waits"].append(("dma", key, val * 16))
                else:
                    Dn = dop["eng"]
                    if Dn == E:
                        if E == "pe" or typ != "raw":
                            continue
                    if waited_seq[E][Dn] >= dop["seq"]:
                        continue
                    waited_seq[E][Dn] = dop["seq"]
                    dop["signal"] = True
                    op["waits"].append(("cmp", d))
        sig_idx = {}
        nsig = {e: 0 for e in self.ENGS}
        for e in self.ENGS:
            for i in self.eng_ops[e]:
                if ops[i]["signal"]:
                    sig_idx[i] = nsig[e]
                    nsig[e] += 1
        stack = contextlib.ExitStack()
        sems = {}
        for e in self.ENGS:
            for g in range((nsig[e] + SIG_R - 1) // SIG_R):
                sems[("cmp", e, g)] = stack.enter_context(nc.semaphore(f"s_{e}_{g}"))
        for name, cnt in self.dma_cnt.items():
            for g in range((cnt + DMA_R - 1) // DMA_R):
                sems[("dma", name, g)] = stack.enter_context(nc.semaphore(f"d_{name}_{g}"))
        self.n_sems = len(sems)
        engmap = {"pe": "tensor", "act": "scalar", "dve": "vector", "pool": "gpsimd", "sp": "sync"}

        def emit_engine(e, eng):
            for i in self.eng_ops[e]:
                op = ops[i]
                for w in op["waits"]:
                    if w[0] == "dma":
                        _, key, val = w
                        eng.wait_ge(sems[("dma", key[0], key[1])], val)
                    else:
                        d = w[1]
                        si = sig_idx[d]
                        eng.wait_ge(sems[("cmp", ops[d]["eng"], si // SIG_R)], si % SIG_R + 1)
                if op["fn"] is None:
                    continue
                inst = op["fn"](eng)
                if inst is None:
                    continue
                if op["dma"] is not None:
                    inst.then_inc(sems[("dma", op["dma"], op["dcount"] // DMA_R)], 16)
                elif op["signal"]:
                    si = sig_idx[i]
                    inst.then_inc(sems[("cmp", e, si // SIG_R)], 1)

        with stack:
            with nc.Block() as block:
                for e in self.ENGS:
                    if not self.eng_ops[e]:
                        continue

                    def _mk(e=e):
                        def _f(eng):
                            emit_engine(e, eng)
                        return _f
                    getattr(block, engmap[e])(_mk())


def _consts():
    c = {}
    c["ident_f"] = np.eye(128, dtype=np.float32)
    c["ident_b"] = np.eye(128, dtype=np.float32).astype(ml_dtypes.bfloat16)
    j = np.arange(128)[:, None]
    i = np.arange(128)[None, :]
    tri = np.zeros((128, 4, 128), np.float32)
    tri[:, 0, :] = (j <= i) * (-1.0 / 16)
    tri[:, 1, :] = (j >= i) * (-1.0 / 16)
    tri[:, 2, :] = (j > i) * (-1.0 / 16)
    tri[:, 3, :] = (j < i) * (-1.0 / 16)
    c["tri"] = tri
    msk = np.zeros((128, 2, 128), np.float32)
    msk[:, 0, :] = (j <= i)
    msk[:, 1, :] = (j >= i)
    c["msk"] = msk
    pm = np.zeros((128, 4, 128), np.float32)
    for gi, w in enumerate((2, 4, 8, 16)):
        for blk in range(2):
            for pos in range(64):
                lo = min(max(pos - w // 2, 0), 64)
                hi = min(max(pos - w // 2 + w, 0), 64)
                cnt = hi - lo
                for jj in range(lo, hi):
                    pm[blk * 64 + jj, gi, blk * 64 + pos] += 1.0 / cnt
                pm[blk * 64 + pos, gi, blk * 64 + pos] -= 1.0
    c["poolm"] = pm.astype(ml_dtypes.bfloat16)
    c["ones_b"] = np.ones((1, 128), np.float32).astype(ml_dtypes.bfloat16)
    return c


DBG_OFFS = {}


class Arena:
    def __init__(self, ten, nbytes):
        self.ten = ten
        self.nbytes = nbytes
        self.off = 0
        self.gen = 0

    def reset(self):
        self.off = 0
        self.gen += 1

    def alloc(self, parts, nelem, dt, name=None):
        if name:
            DBG_OFFS[name] = (self.off, nelem, dt == F32)
        esz = 4 if dt == F32 else 2
        nb = (nelem * esz + 63) // 64 * 64
        assert self.off + nb <= self.nbytes, (self.off, nb, self.nbytes)
        a = self.ten[0:parts, self.off // 2:(self.off + nelem * esz) // 2]
        self.off += nb
        if dt == F32:
            a = a.bitcast(F32)
        return a


def build_program(layers=(0, 1), do_final=True, nseq=SEQ_PER_CORE, stop_after=None):
    nc = bass.Bass("TRN2", target_bir_lowering=False)

    def din(name, shape, dt=F32):
        return nc.dram_tensor(name, list(shape), dt, kind="ExternalInput").ap()

    def dint(name, shape, dt):
        return nc.dram_tensor(name, list(shape), dt, kind="Internal").ap()

    x4 = din("x4", [nseq, T, D])
    ctx4 = din("ctx4", [nseq, CTX, D])
    ccT = din("ccT", [D, 5])
    norm1_g = din("norm1_g", [2, D])
    norm2_g = din("norm2_g", [2, D])
    w_mod = din("w_mod", [2, D, 6 * D])
    b_mod = din("b_mod", [2, 6 * D])
    w_in = din("gla_w_in", [D, 3072])
    w_ga = din("gla_w_gate_a", [2, D, 16])
    w_gb = din("gla_w_gate_b", [2, 16, 512])
    b_g = din("gla_b_gate", [2, 512])
    gnorm = din("gla_norm_g", [1, 256])
    w_out = din("gla_w_out", [D, D])
    pool_w = din("pool_w", [4, 256, 256])
    pool_b = din("pool_b", [1, D])
    pool_scale = din("pool_scale", [1, D])
    w_router = din("w_router", [D, NE])
    b_router = din("b_router", [1, NE])
    w_gate_e = din("w_gate_e", [2, NE, D, DE])
    w_up_e = din("w_up_e", [2, NE, D, DE])
    w_down_e = din("w_down_e", [2, NE, DE, D])
    final_g = din("final_g", [1, D])
    ident_f = din("ident_f", [128, 128])
    ident_b = din("ident_b", [128, 128], BF16)
    tri_d = din("tri", [128, 4, 128])
    msk_d = din("msk", [128, 2, 128])
    poolm_d = din("poolm", [128, 4, 128], BF16)
    ones_d = din("ones_b", [1, 128], BF16)
    out4 = nc.dram_tensor("out4", [nseq, T, D], F32, kind="ExternalOutput").ap()

    WG = dint("WGs", [2, NE, D, DE], BF16)
    WU = dint("WUs", [2, NE, D, DE], BF16)
    WD = dint("WDs", [2, NE, DE, D], BF16)
    WIN = dint("WINs", [D, 3072], BF16)
    WOUT = dint("WOUTs", [D, D], BF16)
    WPOOL = dint("WPOOLs", [4, 256, 256], BF16)
    WGAs = dint("WGAs", [D, 32], BF16)
    MODS = dint("MODS", [2, 5, 6 * D], F32)

    st = contextlib.ExitStack()

    def sb(name, shape, dt):
        return st.enter_context(nc.sbuf_tensor(name, list(shape), dt))

    X = sb("X", [128, 16, D], F32)
    HT = sb("HT", [128, 8, T], BF16)
    ARENA_BYTES = 70 * 1024
    ARENA_T = sb("ARENA", [128, ARENA_BYTES // 2], BF16)
    ar = Arena(ARENA_T, ARENA_BYTES)
    BC = [sb(f"BC{i}", [128, D], F32) for i in range(3)]
    IDF = sb("IDF", [128, 128], F32)
    IDB = sb("IDB", [128, 128], BF16)
    TRI = sb("TRI", [128, 4, 128], F32)
    MSK = sb("MSK", [128, 2, 128], F32)
    POOLM = sb("POOLM", [128, 4, 128], BF16)
    ONES = sb("ONES", [1, 128], BF16)
    PBR = sb("PBR", [1, D], BF16)
    WR = sb("WR", [128, 8, NE], F32)
    BRB = sb("BRB", [128, 16, NE], F32)
    WGA = sb("WGA", [128, 8, 32], BF16)
    WGB = sb("WGB", [64, D], BF16)
    NGB = sb("NGB", [128, 256], F32)
    WP = sb("WP", [128, 4, 2, 256], BF16)
    SSQ = sb("SSQ", [128, 18], F32)
    SQT = sb("SQT", [128, 18], F32)
    RSTD = sb("RSTD", [128, 18], F32)
    SC = sb("SC", [128, 16, NE], F32)
    GATES = sb("GATES", [128, 16, NE], F32)
    RT = [sb(f"RT{i}", [128, 16, NE], F32) for i in range(4)]
    RS = [sb(f"RS{i}", [128, 64], F32) for i in range(4)]
    PS = [st.enter_context(nc.psum_tensor(f"PS{i}", [128, 512], F32)) for i in range(8)]

    p = Prog(nc)
    uid = [0]

    def U():
        uid[0] += 1
        return uid[0]

    rot = {}

    def dma(eng, out, in_, reads, writes, name, nrot=1):
        if nrot > 1:
            k = rot.get(name, 0)
            rot[name] = k + 1
            name = f"{name}_{k % nrot}"
        p.add(eng, lambda e: e.dma_start(out=out, in_=in_), reads=reads, writes=writes, dma=name)

    def pk(i, lo=0, hi=512):
        return [("PS", i, c) for c in range(lo // 128, (hi + 127) // 128)]

    def mm_group(out, pairs, reads, writes, f32=False):
        n = len(pairs)

        def fn(e):
            inst = None
            for q, (l, r) in enumerate(pairs):
                inst = e.matmul(out, lhsT=l, rhs=r, start=(q == 0), stop=(q == n - 1))
            return inst
        p.add("pe", fn, reads=reads, writes=writes)

    dma("sp", IDF[:], ident_f, [], ["IDF"], "c0", 8)
    dma("sp", IDB[:], ident_b, [], ["IDB"], "c0", 8)
    dma("sp", TRI[:], tri_d, [], ["TRI"], "c0", 8)
    dma("sp", MSK[:], msk_d, [], ["MSK"], "c0", 8)
    dma("sp", POOLM[:], poolm_d, [], ["POOLM"], "c0", 8)
    dma("sp", ONES[:], ones_d, [], ["ONES"], "c0", 8)
    dma("sp", WR[:], w_router.rearrange("(kc p) n -> p kc n", p=128), [], ["WR"], "c0", 8)
    dma("sp", NGB[:], gnorm.partition_broadcast(128), [], ["NGB"], "c0", 8)
    dma("sp", BRB[:, 0, :], b_router.partition_broadcast(128), [], ["BRB0"], "c0", 8)
    for b in range(1, 16):
        p.add("dve", lambda e, b=b: e.tensor_copy(out=BRB[:, b, :], in_=BRB[:, 0, :]), reads=["BRB0"], writes=[("BRB", b)])
    brb_keys = ["BRB0"] + [("BRB", b) for b in range(1, 16)]
    if 0 in layers:
        dma("pool", WIN, w_in, [], ["WIN"], "cast", 8)
        dma("pool", WOUT, w_out, [], ["WOUT"], "cast", 8)
        for z in range(2):
            dma("pool", WGAs[:, z * 16:(z + 1) * 16], w_ga[z], [], [("WGAs", z)], "cast", 8)
        p.add("pool", lambda e: e.memset(WGB[:], 0.0), writes=["WGB"])
        for z in range(2):
            dma("pool", WGB[z * 16:(z + 1) * 16, z * 512:(z + 1) * 512], w_gb[z], ["WGB"], [("WGBp", z)], "cast", 8)
            dma("pool", WGB[32:33, z * 512:(z + 1) * 512], b_g[z:z + 1, :], ["WGB"], [("WGBb", z)], "cast", 8)
        wgb_keys = ["WGB"] + [("WGBp", z) for z in range(2)] + [("WGBb", z) for z in range(2)]
        dma("sp", WGA[:], WGAs.rearrange("(kc p) n -> p kc n", p=128), [("WGAs", 0), ("WGAs", 1)], ["WGA"], "c0", 8)
    if 1 in layers:
        dma("pool", WPOOL, pool_w, [], ["WPOOL"], "cast", 8)
        dma("pool", PBR[:], pool_b, [], ["PBR"], "cast", 8)
        dma("sp", WP[:], WPOOL.rearrange("g (kc p) n -> p g kc n", p=128), ["WPOOL"], ["WP"], "c0", 8)
    for li in layers:
        if os.environ.get("K_DBG_NOEXP") == "1":
            break
        for e_ in range(NE):
            dma("pool", WG[li, e_], w_gate_e[li, e_], [], [("WG", li, e_)], "cast", 8)
            dma("pool", WU[li, e_], w_up_e[li, e_], [], [("WU", li, e_)], "cast", 8)
            dma("pool", WD[li, e_], w_down_e[li, e_], [], [("WD", li, e_)], "cast", 8)

    ar.reset()
    CC = ar.alloc(128, 8 * 5, F32).rearrange("p (a b) -> p a b", a=8)
    SCC = ar.alloc(128, 8 * 5, F32).rearrange("p (a b) -> p a b", a=8)
    WM = [ar.alloc(128, 8 * 512, F32).rearrange("p (a b) -> p a b", a=8) for _ in range(2)]
    BMT = [ar.alloc(5, 512, F32) for _ in range(2)]
    NGT = [ar.alloc(5, 512, F32) for _ in range(2)]
    MT = [ar.alloc(5, 512, F32) for _ in range(2)]
    dma("sp", CC, ccT.rearrange("(kc p) n -> p kc n", p=128), [], ["CC"], "c0", 8)
    p.add("act", lambda e: e.activation(out=SCC, in_=CC, func=AF.Silu), reads=["CC"], writes=["SCC"])
    q = 0
    for li in layers:
        for ct in range(12):
            b = q % 2
            q += 1
            dma("sp", WM[b], w_mod[li][:, ct * 512:(ct + 1) * 512].rearrange("(kc p) n -> p kc n", p=128), [], [("WM", b)], f"wm{b}", 3)
            dma("sp", BMT[b], b_mod[li:li + 1, ct * 512:(ct + 1) * 512].partition_broadcast(5), [], [("BMT", b)], f"wm{b}", 3)
            is_sc = ct in (2, 3, 8, 9)
            if is_sc:
                ng = norm1_g if ct < 6 else norm2_g
                dma("sp", NGT[b], ng[li:li + 1, (ct % 2) * 512:(ct % 2 + 1) * 512].partition_broadcast(5), [], [("NGT", b)], f"wm{b}", 3)
            mm_group(PS[b][0:5, :], [(SCC[:, kc, :], WM[b][:, kc, :]) for kc in range(8)],
                     reads=["SCC", ("WM", b)], writes=pk(b))
            p.add("dve", lambda e, b=b: e.tensor_tensor(out=MT[b], in0=PS[b][0:5, :], in1=BMT[b], op=ALU.add),
                  reads=pk(b) + [("BMT", b)], writes=[("MT", b)])
            if is_sc:
                p.add("dve", lambda e, b=b: e.scalar_tensor_tensor(out=MT[b], in0=MT[b], scalar=1.0, in1=NGT[b], op0=ALU.add, op1=ALU.mult),
                      reads=[("MT", b), ("NGT", b)], writes=[("MT", b)])
            dma("sp", MODS[li, :, ct * 512:(ct + 1) * 512], MT[b], [("MT", b)], [("MODS", li, ct)], f"mt{b}")

    def mods_row(li, row, part):
        return MODS[li, row:row + 1, part * D:(part + 1) * D].partition_broadcast(128)

    def mods_keys(li, part):
        return [("MODS", li, 2 * part), ("MODS", li, 2 * part + 1)]

    def load_bc(i, src, reads):
        dma("sp", BC[i][:], src, reads, [("BC", i)], f"bc{i}")

    def rms_stats(srcs, scale):
        n = len(srcs)
        junk = ar.alloc(128, D, F32)
        ju = U()
        for i, (a, rk) in enumerate(srcs):
            w = a.shape[-1]
            p.add("act", lambda e, a=a, i=i, w=w: e.activation(out=junk[:, 0:w], in_=a, func=AF.Square, accum_out=SSQ[:, i:i + 1]),
                  reads=rk, writes=[("junk", ju), ("SSQ", i)])
        p.add("act", lambda e: e.activation(out=SQT[:, 0:n], in_=SSQ[:, 0:n], func=AF.Sqrt, bias=EPS, scale=scale),
              reads=[("SSQ", i) for i in range(n)], writes=["SQT"])
        p.add("dve", lambda e: e.reciprocal(out=RSTD[:, 0:n], in_=SQT[:, 0:n]), reads=["SQT"], writes=["RSTD"])

    def xk(blk, half=None):
        if half is None:
            return [("X", blk, 0), ("X", blk, 1)]
        return [("X", blk, half)]

    for s in range(nseq):
        if stop_after == "pro":
            break
        for g4 in range(4):
            dma("sp", X[:, g4 * 4:(g4 + 1) * 4, :], x4[s, g4 * 512:(g4 + 1) * 512, :].rearrange("(b p) d -> p b d", p=128),
                [], [k for b in range(g4 * 4, g4 * 4 + 4) for k in xk(b)], "x", 4)
        for li in layers:
            p.barrier()
            ar.reset()
            if li == 0:
                HCT = ar.alloc(128, 8 * CTX, BF16, name='HCT').rearrange("p (a b) -> p a b", a=8)
                mk = ar.off
                CX = ar.alloc(128, 2 * D, F32, name='CX').rearrange("p (a b) -> p a b", a=2)
                T1 = [ar.alloc(128, D, F32, name=f'T1_{_i}') for _i in range(2)]
                HB = [ar.alloc(128, D, BF16, name=f'HB_{_i}') for _i in range(2)]
                dma("sp", CX, ctx4[s].rearrange("(b p) d -> p b d", p=128), [], ["CX"], "cx")
                load_bc(0, mods_row(0, s, 1), mods_keys(0, 1))
                load_bc(1, mods_row(0, s, 0), mods_keys(0, 0))
                rms_stats([(X[:, b, :], xk(b)) for b in range(16)] + [(CX[:, b, :], ["CX"]) for b in range(2)], 1.0 / D)
                def n1a(blk):
                    if blk == 16:
                        load_bc(0, mods_row(0, 4, 1), mods_keys(0, 1))
                        load_bc(1, mods_row(0, 4, 0), mods_keys(0, 0))
                    b2 = blk % 2
                    src = X[:, blk, :] if blk < 16 else CX[:, blk - 16, :]
                    srck = xk(blk) if blk < 16 else ["CX"]
                    p.add("dve", lambda e: e.scalar_tensor_tensor(
                        out=T1[b2], in0=src, scalar=RSTD[:, blk:blk + 1], in1=BC[0][:], op0=ALU.mult, op1=ALU.mult),
                        reads=srck + ["RSTD", ("BC", 0)], writes=[("T1", b2)])
                    p.add("pool", lambda e: e.tensor_tensor(out=HB[b2], in0=T1[b2], in1=BC[1][:], op=ALU.add),
                          reads=[("T1", b2), ("BC", 1)], writes=[("HB", b2)])

                def n1b(blk):
                    b2 = blk % 2
                    for hh in range(2):
                        pb = b2 * 2 + hh
                        for kc4 in range(4):
                            kc = hh * 4 + kc4
                            mm_group(PS[pb][:, kc4 * 128:(kc4 + 1) * 128], [(HB[b2][:, kc * 128:(kc + 1) * 128], IDB[:])],
                                     reads=[("HB", b2), "IDB"], writes=pk(pb, kc4 * 128, kc4 * 128 + 128))
                        if blk < 16:
                            dst = HT[:, hh * 4:(hh + 1) * 4, blk * 128:(blk + 1) * 128]
                            dk = [("HT", blk)]
                        else:
                            dst = HCT[:, hh * 4:(hh + 1) * 4, (blk - 16) * 128:(blk - 15) * 128]
                            dk = [("HCT", blk - 16)]
                        p.add("act", lambda e, dst=dst, pb=pb: e.activation(out=dst, in_=PS[pb][:].rearrange("p (a b) -> p a b", a=4), func=AF.Copy),
                              reads=pk(pb), writes=dk)

                for it in range(19):
                    if it < 18:
                        n1a(it)
                    if it >= 1:
                        n1b(it - 1)
                if stop_after == "n1":
                    break
                p.barrier()
                ar.off = mk
                load_bc(2, mods_row(0, s, 2), mods_keys(0, 2))
                LOWT = ar.alloc(64, 2304, BF16)
                WINH = ar.alloc(128, 8 * 768, BF16).rearrange("p (a b) -> p a b", a=8)
                WOUTH = ar.alloc(128, 2 * D, BF16).rearrange("p (a b) -> p a b", a=2)
                OBUF = ar.alloc(128, 16 * 256, F32).rearrange("p (a b) -> p a b", a=16)
                E1 = [ar.alloc(128, 128, F32) for _ in range(2)]
                SP_ = [ar.alloc(128, 128, F32, name=f'SP{_i}') for _i in range(2)]
                EQR = [ar.alloc(128, 256, F32, name=f'EQR{_i}') for _i in range(3)]
                EK = [ar.alloc(128, 128, F32) for _ in range(2)]
                QT = [ar.alloc(128, 128, BF16) for _ in range(3)]
                KT = [ar.alloc(128, 128, BF16) for _ in range(3)]
                KH = [ar.alloc(128, 128, BF16) for _ in range(3)]
                VB = [ar.alloc(128, 256, BF16) for _ in range(3)]
                ATM = [ar.alloc(128, 128, BF16) for _ in range(2)]
                SS = [ar.alloc(128, 256, F32, name=f'SS{_i}') for _i in range(2)]
                SBF = [[ar.alloc(128, 256, BF16) for _ in range(2)] for _ in range(2)]
                SR = [ar.alloc(128, 256, F32) for _ in range(2)]
                TT = [ar.alloc(128, 256, F32) for _ in range(2)]
                OGB = [ar.alloc(128, 256, BF16) for _ in range(2)]
                OGT2 = [ar.alloc(128, 256, BF16).rearrange("p (a b) -> p a b", a=2) for _ in range(2)]
                TMP = [ar.alloc(128, 512, F32) for _ in range(4)]

                def hT(c, kc):
                    if c < 16:
                        return HT[:, kc, c * 128:(c + 1) * 128], ("HT", c)
                    return HCT[:, kc, (c - 16) * 128:(c - 15) * 128], ("HCT", c - 16)

                p.add("pool", lambda e: e.memset(LOWT, 1.0), writes=["LOWTm"])
                for tt in range(5):
                    if tt < 4:
                        rh = [HT[:, kc, tt * 512:(tt + 1) * 512] for kc in range(8)]
                        rk = [("HT", b) for b in range(tt * 4, tt * 4 + 4)]
                        w = 512
                    else:
                        rh = [HCT[:, kc, :] for kc in range(8)]
                        rk = [("HCT", 0), ("HCT", 1)]
                        w = 256
                    pb = tt % 2
                    mm_group(PS[pb][0:32, 0:w], [(WGA[:, kc, :], rh[kc]) for kc in range(8)], reads=rk + ["WGA"], writes=pk(pb, 0, w))
                    p.add("act", lambda e, tt=tt, w=w, pb=pb: e.activation(out=LOWT[0:32, tt * 512:tt * 512 + w], in_=PS[pb][0:32, 0:w], func=AF.Copy),
                          reads=pk(pb, 0, w) + ["LOWTm"], writes=[("LOWT", tt)])

                if stop_after == "low":
                    break
                stopped = False
                for h in range(4):
                    if stop_after == ("head", h):
                        stopped = True
                        break
                    hu = U()
                    dma("sp", WINH[:, :, 0:128], WIN[:, h * 128:(h + 1) * 128].rearrange("(kc p) n -> p kc n", p=128), ["WIN"], [("WINH", 0)], "winh", 5)
                    dma("sp", WINH[:, :, 128:256], WIN[:, 512 + h * 128:512 + (h + 1) * 128].rearrange("(kc p) n -> p kc n", p=128), ["WIN"], [("WINH", 1)], "winh", 5)
                    dma("sp", WINH[:, :, 256:512], WIN[:, 1024 + h * 256:1024 + (h + 1) * 256].rearrange("(kc p) n -> p kc n", p=128), ["WIN"], [("WINH", 2)], "winh", 5)
                    dma("sp", WINH[:, :, 512:768], WIN[:, 2048 + h * 256:2048 + (h + 1) * 256].rearrange("(kc p) n -> p kc n", p=128), ["WIN"], [("WINH", 3)], "winh", 5)
                    dma("sp", WOUTH, WOUT[h * 256:(h + 1) * 256, :].rearrange("(kc p) n -> p kc n", p=128), ["WOUT"], ["WOUTH"], "winh", 5)
                    for dr in range(2):
                        p.add("pool", lambda e, dr=dr: e.memset(SS[dr], 0.0), writes=[("SS", dr)])
                        p.add("pool", lambda e, dr=dr: e.memset(SBF[dr][0], 0.0), writes=[("SBF", dr, 0)])
                    order = [[16, 17] + list(range(16)), [17, 16] + list(range(15, -1, -1))]
                    steps = []
                    for i in range(18):
                        steps.append((0, order[0][i], i))
                        steps.append((1, order[1][i], i))

                    def S1(k, dr, c, i):
                        r2 = k % 2
                        A0, A1, ZB = 3 * r2, 3 * r2 + 1, 3 * r2 + 2
                        hts = [hT(c, kc) for kc in range(8)]
                        hk = [hts[0][1]]
                        zc = dr * 512 + h * 128
                        mm_group(PS[ZB][:, 0:128], [(LOWT[0:64, c * 128:(c + 1) * 128], WGB[0:64, zc:zc + 128])],
                                 reads=[("LOWT", c // 4), "LOWTm"] + wgb_keys, writes=pk(ZB))
                        p.add("act", lambda e: e.activation(out=E1[r2], in_=PS[ZB][:, 0:128], func=AF.Exp, scale=-1.0),
                              reads=pk(ZB), writes=[("E1", r2)])
                        p.add("act", lambda e: e.activation(out=SP_[r2], in_=E1[r2], func=AF.Ln, bias=1.0),
                              reads=[("E1", r2)], writes=[("SP", r2)])
                        mm_group(PS[A0][:, 0:384], [(hts[kc][0], WINH[:, kc, 128:512]) for kc in range(8)],
                                 reads=hk + [("WINH", 1), ("WINH", 2)], writes=pk(A0))
                        mm_group(PS[A0][:, 384:512], [(WINH[:, kc, 128:256], hts[kc][0]) for kc in range(8)],
                                 reads=hk + [("WINH", 1)], writes=pk(A0))
                        mm_group(PS[A1][:, 0:128], [(WINH[:, kc, 0:128], hts[kc][0]) for kc in range(8)],
                                 reads=hk + [("WINH", 0)], writes=pk(A1))

                    def S2(k, dr, c, i):
                        r2 = k % 2
                        r3 = k % 3
                        A0, A1, ZB = 3 * r2, 3 * r2 + 1, 3 * r2 + 2
                        mm_group(PS[ZB][:, 128:256], [(SP_[r2], TRI[:, dr, :])], reads=[("SP", r2), "TRI"], writes=pk(ZB))
                        mm_group(PS[ZB][:, 256:384], [(TRI[:, 2 + dr, :], SP_[r2])], reads=[("SP", r2), "TRI"], writes=pk(ZB))
                        p.add("act", lambda e: e.activation(out=EQR[r3], in_=PS[ZB][:, 128:384], func=AF.Exp),
                              reads=pk(ZB), writes=[("EQR", r3)])
                        p.add("act", lambda e: e.activation(out=EK[r2], in_=PS[ZB][:, 128:256], func=AF.Exp, scale=-1.0),
                              reads=pk(ZB), writes=[("EK", r2)])
                        p.add("act", lambda e: e.activation(out=VB[r3], in_=PS[A0][:, 128:384], func=AF.Copy),
                              reads=pk(A0), writes=[("VB", r3)])
                        p.add("dve", lambda e: e.scalar_tensor_tensor(out=QT[r3], in0=PS[A1][:, 0:128], scalar=float(128 ** -0.5), in1=EQR[r3][:, 0:128],
                                                                      op0=ALU.mult, op1=ALU.mult),
                              reads=pk(A1) + [("EQR", r3)], writes=[("QT", r3)])
                        p.add("dve", lambda e: e.tensor_tensor(out=KT[r3], in0=PS[A0][:, 384:512], in1=EK[r2], op=ALU.mult),
                              reads=pk(A0) + [("EK", r2)], writes=[("KT", r3)])
                        p.add("dve", lambda e: e.tensor_tensor(out=KH[r3], in0=PS[A0][:, 0:128], in1=EQR[r3][:, 128:256], op=ALU.mult),
                              reads=pk(A0) + [("EQR", r3)], writes=[("KH", r3)])

                    def S3(k, dr, c, i):
                        r2 = k % 2
                        r3 = k % 3
                        A1 = 3 * r2 + 1
                        mm_group(PS[A1][:, 128:256], [(KT[r3], QT[r3])], reads=[("KT", r3), ("QT", r3)], writes=pk(A1))
                        p.add("dve", lambda e: e.tensor_tensor(out=ATM[r2], in0=PS[A1][:, 128:256], in1=MSK[:, dr, :], op=ALU.mult),
                              reads=pk(A1) + ["MSK"], writes=[("ATM", r2)])

                    def S4(k, dr, c, i):
                        r2 = k % 2
                        r3 = k % 3
                        BO = 6 + dr
                        sb_cur = SBF[dr][i % 2]
                        sb_nxt = SBF[dr][(i + 1) % 2]
                        if c < 16:
                            mm_group(PS[BO][:, 0:256], [(ATM[r2], VB[r3]), (QT[r3], sb_cur)],
                                     reads=[("ATM", r2), ("VB", r3), ("QT", r3), ("SBF", dr, i % 2)], writes=pk(BO))
                        mm_group(PS[BO][:, 256:512], [(KH[r3], VB[r3])], reads=[("KH", r3), ("VB", r3)], writes=pk(BO))
                        if c < 16:
                            first = (dr == 0) if c <= 7 else (dr == 1)
                            if first:
                                p.add("act", lambda e: e.activation(out=OBUF[:, c, :], in_=PS[BO][:, 0:256], func=AF.Copy),
                                      reads=pk(BO), writes=[("OBUF", c)])
                            else:
                                p.add("dve", lambda e: e.tensor_tensor(out=OBUF[:, c, :], in0=PS[BO][:, 0:256], in1=OBUF[:, c, :], op=ALU.add),
                                      reads=pk(BO) + [("OBUF", c)], writes=[("OBUF", c)])
                        edge = 127 if dr == 0 else 0
                        p.add("dve", lambda e: e.scalar_tensor_tensor(out=SS[dr], in0=SS[dr], scalar=EQR[r3][:, edge:edge + 1], in1=PS[BO][:, 256:512],
                                                                      op0=ALU.mult, op1=ALU.add),
                              reads=[("SS", dr), ("EQR", r3)] + pk(BO), writes=[("SS", dr)])
                        p.add("pool", lambda e: e.tensor_copy(out=sb_nxt, in_=SS[dr]), reads=[("SS", dr)], writes=[("SBF", dr, (i + 1) % 2)])

                    nst = len(steps)
                    for it in range(nst + 3):
                        if it < nst:
                            S1(it, *steps[it])
                        if 0 <= it - 1 < nst:
                            S2(it - 1, *steps[it - 1])
                        if 0 <= it - 2 < nst:
                            S3(it - 2, *steps[it - 2])
                        if 0 <= it - 3 < nst:
                            S4(it - 3, *steps[it - 3])

                    if stop_after == ("scan", h):
                        stopped = True
                        break
                    junk = TT[0]
                    for c in range(16):
                        p.add("act", lambda e, c=c: e.activation(out=junk, in_=OBUF[:, c, :], func=AF.Square, accum_out=SSQ[:, c:c + 1]),
                              reads=[("OBUF", c)], writes=[("TT", 0), ("SSQ", c)])
                    p.add("act", lambda e: e.activation(out=SQT[:, 0:16], in_=SSQ[:, 0:16], func=AF.Sqrt, bias=EPS, scale=1.0 / 256),
                          reads=[("SSQ", i) for i in range(16)], writes=["SQT"])
                    p.add("dve", lambda e: e.reciprocal(out=RSTD[:, 0:16], in_=SQT[:, 0:16]), reads=["SQT"], writes=["RSTD"])
                    def ro1(c):
                        r = c % 2
                        b0 = 4 * r
                        hts = [hT(c, kc) for kc in range(8)]
                        mm_group(PS[b0][:, 0:256], [(hts[kc][0], WINH[:, kc, 512:768]) for kc in range(8)],
                                 reads=[hts[0][1], ("WINH", 3)], writes=pk(b0, 0, 256))
                        p.add("act", lambda e: e.activation(out=SR[r], in_=PS[b0][:, 0:256], func=AF.Silu),
                              reads=pk(b0, 0, 256), writes=[("SR", r)])
                        p.add("dve", lambda e: e.scalar_tensor_tensor(out=TT[r], in0=OBUF[:, c, :], scalar=RSTD[:, c:c + 1], in1=NGB[:],
                                                                      op0=ALU.mult, op1=ALU.mult),
                              reads=[("OBUF", c), "RSTD", "NGB"], writes=[("TT", r)])
                        p.add("pool", lambda e: e.tensor_tensor(out=OGB[r], in0=TT[r], in1=SR[r], op=ALU.mult),
                              reads=[("TT", r), ("SR", r)], writes=[("OGB", r)])

                    def ro2(c):
                        r = c % 2
                        b0 = 4 * r
                        for fc in range(2):
                            mm_group(PS[b0 + 3][:, fc * 128:(fc + 1) * 128], [(OGB[r][:, fc * 128:(fc + 1) * 128], IDB[:])],
                                     reads=[("OGB", r), "IDB"], writes=pk(b0 + 3))
                        p.add("act", lambda e: e.activation(out=OGT2[r], in_=PS[b0 + 3][:, 0:256].rearrange("p (a b) -> p a b", a=2), func=AF.Copy),
                              reads=pk(b0 + 3), writes=[("OGT2", r)])

                    def ro3(c):
                        r = c % 2
                        b0 = 4 * r
                        for half in range(2):
                            pb = b0 + 1 + half
                            ti = r * 2 + half
                            mm_group(PS[pb][:, :], [(OGT2[r][:, kc2, :], WOUTH[:, kc2, half * 512:(half + 1) * 512]) for kc2 in range(2)],
                                     reads=[("OGT2", r), "WOUTH"], writes=pk(pb))
                            p.add("dve", lambda e, pb=pb, ti=ti, half=half: e.tensor_tensor(out=TMP[ti], in0=PS[pb][:, :], in1=BC[2][:, half * 512:(half + 1) * 512], op=ALU.mult),
                                  reads=pk(pb) + [("BC", 2)], writes=[("TMP", ti)])
                            p.add("pool", lambda e, ti=ti, half=half: e.tensor_tensor(out=X[:, c, half * 512:(half + 1) * 512], in0=X[:, c, half * 512:(half + 1) * 512], in1=TMP[ti], op=ALU.add),
                                  reads=[("TMP", ti)] + xk(c, half), writes=xk(c, half))

                    for c in range(-1, 17):
                        if 0 <= c + 1 < 16:
                            ro1(c + 1)
                        if 0 <= c < 16:
                            ro2(c)
                        if 0 <= c - 1 < 16:
                            ro3(c - 1)
                if stopped:
                    break
            else:
                T1p = [ar.alloc(128, D, F32) for _ in range(2)]
                HBp = [ar.alloc(128, D, BF16) for _ in range(2)]
                PT = [ar.alloc(128, 8 * 128, BF16).rearrange("p (a b) -> p a b", a=8) for _ in range(2)]
                TMPY = [ar.alloc(128, D, F32) for _ in range(2)]
                load_bc(0, mods_row(1, s, 1), mods_keys(1, 1))
                load_bc(1, mods_row(1, s, 0), mods_keys(1, 0))
                load_bc(2, mods_row(1, s, 2), mods_keys(1, 2))
                dma("sp", TMPY[0], pool_scale.partition_broadcast(128), [], [("TMPY", 0)], "psc")
                p.add("dve", lambda e: e.tensor_tensor(out=BC[2][:], in0=BC[2][:], in1=TMPY[0], op=ALU.mult),
                      reads=[("BC", 2), ("TMPY", 0)], writes=[("BC", 2)])
                rms_stats([(X[:, b, :], xk(b)) for b in range(16)], 1.0 / D)
                def pma(blk):
                    r = blk % 2
                    p.add("dve", lambda e: e.scalar_tensor_tensor(
                        out=T1p[r], in0=X[:, blk, :], scalar=RSTD[:, blk:blk + 1], in1=BC[0][:], op0=ALU.mult, op1=ALU.mult),
                        reads=xk(blk) + ["RSTD", ("BC", 0)], writes=[("T1", r)])
                    p.add("pool", lambda e: e.tensor_tensor(out=HBp[r], in0=T1p[r], in1=BC[1][:], op=ALU.add),
                          reads=[("T1", r), ("BC", 1)], writes=[("HB", r)])

                def pmb(blk):
                    r = blk % 2
                    b0 = 4 * r
                    for hh in range(2):
                        pb = b0 + hh
                        for kc4 in range(4):
                            fc = hh * 4 + kc4
                            mm_group(PS[pb][:, kc4 * 128:(kc4 + 1) * 128], [(HBp[r][:, fc * 128:(fc + 1) * 128], POOLM[:, fc // 2, :])],
                                     reads=[("HB", r), "POOLM"], writes=pk(pb, kc4 * 128, kc4 * 128 + 128))
                        p.add("act", lambda e, hh=hh, pb=pb: e.activation(out=PT[r][:, hh * 4:(hh + 1) * 4, :], in_=PS[pb][:].rearrange("p (a b) -> p a b", a=4), func=AF.Copy),
                              reads=pk(pb), writes=[("PT", r, hh)])

                def pmc(blk):
                    r = blk % 2
                    b0 = 4 * r
                    for g in range(4):
                        pb = b0 + 2 + g // 2
                        lo = (g % 2) * 256
                        mm_group(PS[pb][:, lo:lo + 256],
                                 [(PT[r][:, 2 * g, :], WP[:, g, 0, :]), (PT[r][:, 2 * g + 1, :], WP[:, g, 1, :]), (ONES[0:1, :], PBR[0:1, g * 256:(g + 1) * 256])],
                                 reads=[("PT", r, g // 2), "WP", "ONES", "PBR"], writes=pk(pb, lo, lo + 256))
                    for half in range(2):
                        pb = b0 + 2 + half
                        p.add("dve", lambda e, half=half, pb=pb: e.tensor_tensor(out=TMPY[r][:, half * 512:(half + 1) * 512], in0=PS[pb][:, :], in1=BC[2][:, half * 512:(half + 1) * 512], op=ALU.mult),
                              reads=pk(pb) + [("BC", 2)], writes=[("TMPY", r)])
                    p.add("pool", lambda e: e.tensor_tensor(out=X[:, blk, :], in0=X[:, blk, :], in1=TMPY[r], op=ALU.add),
                          reads=[("TMPY", r)] + xk(blk), writes=xk(blk))

                for it in range(18):
                    if it < 16:
                        pma(it)
                    if 0 <= it - 1 < 16:
                        pmb(it - 1)
                    if 0 <= it - 2 < 16:
                        pmc(it - 2)
            if stop_after == ("mix", li):
                break
            p.barrier()
            ar.reset()
            T1n = [ar.alloc(128, D, F32) for _ in range(3)]
            H2F = [ar.alloc(128, 8 * 128, F32).rearrange("p (a b) -> p a b", a=8) for _ in range(2)]
            load_bc(0, mods_row(li, s, 4), mods_keys(li, 4))
            load_bc(1, mods_row(li, s, 3), mods_keys(li, 3))
            rms_stats([(X[:, b, :], xk(b)) for b in range(16)], 1.0 / D)
            def n2a(blk):
                r3 = blk % 3
                p.add("dve", lambda e: e.scalar_tensor_tensor(
                    out=T1n[r3], in0=X[:, blk, :], scalar=RSTD[:, blk:blk + 1], in1=BC[0][:], op0=ALU.mult, op1=ALU.mult),
                    reads=xk(blk) + ["RSTD", ("BC", 0)], writes=[("T1", r3)])
                p.add("pool", lambda e: e.tensor_tensor(out=T1n[r3], in0=T1n[r3], in1=BC[1][:], op=ALU.add),
                      reads=[("T1", r3), ("BC", 1)], writes=[("T1", r3)])

            def n2b(blk):
                r3 = blk % 3
                r = blk % 2
                b0 = 4 * r
                for hh in range(2):
                    pb = b0 + hh
                    for kc4 in range(4):
                        kc = hh * 4 + kc4
                        mm_group(PS[pb][:, kc4 * 128:(kc4 + 1) * 128], [(T1n[r3][:, kc * 128:(kc + 1) * 128], IDF[:])],
                                 reads=[("T1", r3), "IDF"], writes=pk(pb, kc4 * 128, kc4 * 128 + 128))
                    p.add("act", lambda e, hh=hh, pb=pb: e.activation(out=HT[:, hh * 4:(hh + 1) * 4, blk * 128:(blk + 1) * 128],
                                                                      in_=PS[pb][:].rearrange("p (a b) -> p a b", a=4), func=AF.Copy),
                          reads=pk(pb), writes=[("HT", blk)] if hh == 0 else [("HTb", blk)])
                    p.add("dve", lambda e, hh=hh, pb=pb: e.tensor_copy(out=H2F[r][:, hh * 4:(hh + 1) * 4, :], in_=PS[pb][:].rearrange("p (a b) -> p a b", a=4)),
                          reads=pk(pb), writes=[("H2F", r, hh)])

            def n2c(blk):
                r = blk % 2
                b0 = 4 * r
                mm_group(PS[b0 + 2][:, 0:NE], [(H2F[r][:, kc, :], WR[:, kc, :]) for kc in range(8)],
                         reads=[("H2F", r, 0), ("H2F", r, 1), "WR"], writes=pk(b0 + 2, 0, 128))
                p.add("act", lambda e: e.activation(out=SC[:, blk, :], in_=PS[b0 + 2][:, 0:NE], func=AF.Sigmoid),
                      reads=pk(b0 + 2, 0, 128), writes=[("SC", blk)])

            for it in range(18):
                if it < 16:
                    n2a(it)
                if 0 <= it - 1 < 16:
                    n2b(it - 1)
                if 0 <= it - 2 < 16:
                    n2c(it - 2)
            sck = [("SC", b) for b in range(16)]
            SEL, CH, WT, _r3 = RT
            THR, M1, GS, TMPR = RS
            GM = SQT[:, 0:16]
            DEN = SSQ[:, 0:16]

            def dv(fn, reads, writes):
                p.add("dve", fn, reads=reads, writes=writes)
            dv(lambda e: e.tensor_tensor(out=SEL[:], in0=SC[:], in1=BRB[:], op=ALU.add), sck + brb_keys, ["SEL"])
            selv = SEL[:].rearrange("p b (g j) -> p (b g) j", j=4)
            a_ = [selv[:, :, j] for j in range(4)]
            pairs = [(0, 1), (0, 2), (0, 3), (1, 2), (1, 3), (2, 3)]
            for qi, (i0, i1) in enumerate(pairs):
                if qi == 0:
                    dv(lambda e, i0=i0, i1=i1: e.tensor_tensor(out=THR[:], in0=a_[i0], in1=a_[i1], op=ALU.min), ["SEL"], ["THR"])
                else:
                    dv(lambda e, i0=i0, i1=i1: e.tensor_tensor(out=TMPR[:], in0=a_[i0], in1=a_[i1], op=ALU.min), ["SEL"], ["TMPR"])
                    dv(lambda e: e.tensor_tensor(out=THR[:], in0=THR[:], in1=TMPR[:], op=ALU.max), ["THR", "TMPR"], ["THR"])
            dv(lambda e: e.tensor_reduce(out=M1[:], in_=selv, axis=AX.X, op=ALU.max), ["SEL"], ["M1"])
            dv(lambda e: e.tensor_tensor(out=GS[:], in0=M1[:], in1=THR[:], op=ALU.add), ["M1", "THR"], ["GS"])
            gsv = GS[:].rearrange("p (b g) -> p b g", g=4)
            dv(lambda e: e.tensor_reduce(out=GM, in_=gsv, axis=AX.X, op=ALU.max), ["GS"], ["GM"])
            isg = M1[:].rearrange("p (b g) -> p b g", g=4)
            dv(lambda e: e.tensor_tensor(out=isg, in0=gsv, in1=GM.unsqueeze(2).broadcast_to([128, 16, 4]), op=ALU.is_equal), ["GS", "GM", "M1"], ["ISG"])
            chv = CH[:].rearrange("p b (g j) -> p (b g) j", j=4)
            dv(lambda e: e.tensor_tensor(out=chv, in0=selv, in1=THR[:].unsqueeze(2).broadcast_to([128, 64, 4]), op=ALU.is_ge), ["SEL", "THR"], ["CH"])
            dv(lambda e: e.tensor_tensor(out=chv, in0=chv, in1=M1[:].unsqueeze(2).broadcast_to([128, 64, 4]), op=ALU.mult), ["CH", "ISG"], ["CH"])
            dv(lambda e: e.tensor_tensor(out=WT[:], in0=SC[:], in1=CH[:], op=ALU.mult), sck + ["CH"], ["WT"])
            dv(lambda e: e.tensor_reduce(out=DEN, in_=WT[:], axis=AX.X, op=ALU.add), ["WT"], ["DEN"])
            dv(lambda e: e.reciprocal(out=GM, in_=DEN), ["DEN", "GM"], ["RDEN"])
            dv(lambda e: e.tensor_tensor(out=GATES[:], in0=WT[:], in1=GM.unsqueeze(2).broadcast_to([128, 16, NE]), op=ALU.mult), ["WT", "RDEN"], ["GATES"])

            p.barrier()
            ar.reset()
            W1 = [ar.alloc(128, 8 * DE, BF16).rearrange("p (a b) -> p a b", a=8) for _ in range(2)]
            W3 = [ar.alloc(128, 8 * DE, BF16).rearrange("p (a b) -> p a b", a=8) for _ in range(2)]
            W2 = [ar.alloc(128, 4 * D, BF16).rearrange("p (a b) -> p a b", a=4) for _ in range(2)]
            HE = [ar.alloc(128, 4 * 512, BF16).rearrange("p (a b) -> p a b", a=4) for _ in range(2)]
            SG = [ar.alloc(128, 512, BF16) for _ in range(2)]
            G2B = ar.alloc(128, D, BF16)
            load_bc(0, mods_row(li, s, 5), mods_keys(li, 5))
            p.add("act", lambda e: e.activation(out=G2B, in_=BC[0][:], func=AF.Copy), reads=[("BC", 0)], writes=["G2B"])
            def moe_gu(ex, t, hb):
                wb = ex % 2
                if t == 0:
                    dma("sp", W1[wb], WG[li, ex].rearrange("(kc p) n -> p kc n", p=128), [("WG", li, ex)], [("W1", wb)], f"w1{wb}")
                    dma("sp", W3[wb], WU[li, ex].rearrange("(kc p) n -> p kc n", p=128), [("WU", li, ex)], [("W3", wb)], f"w3{wb}")
                    dma("sp", W2[wb], WD[li, ex].rearrange("(kc p) n -> p kc n", p=128), [("WD", li, ex)], [("W2", wb)], f"w2{wb}")
                    p.add("pool", lambda e: e.tensor_tensor(out=W2[wb], in0=W2[wb], in1=G2B.unsqueeze(1).broadcast_to([128, 4, D]), op=ALU.mult),
                          reads=[("W2", wb), "G2B"], writes=[("W2", wb)])
                htk = [("HT", b_) for b_ in range(t * 4, t * 4 + 4)] + [("HTb", b_) for b_ in range(t * 4, t * 4 + 4)]
                for m in range(4):
                    pg = m % 2
                    pu = 2 + m % 2
                    mm_group(PS[pg][:, :], [(W1[wb][:, kc, m * 128:(m + 1) * 128], HT[:, kc, t * 512:(t + 1) * 512]) for kc in range(8)],
                             reads=htk + [("W1", wb)], writes=pk(pg))
                    mm_group(PS[pu][:, :], [(W3[wb][:, kc, m * 128:(m + 1) * 128], HT[:, kc, t * 512:(t + 1) * 512]) for kc in range(8)],
                             reads=htk + [("W3", wb)], writes=pk(pu))
                    p.add("act", lambda e, pg=pg: e.activation(out=SG[pg], in_=PS[pg][:, :], func=AF.Silu), reads=pk(pg), writes=[("SG", pg)])
                    p.add("dve", lambda e, pg=pg, pu=pu, m=m: e.tensor_tensor(out=HE[hb][:, m, :], in0=PS[pu][:, :], in1=SG[pg], op=ALU.mult),
                          reads=pk(pu) + [("SG", pg)], writes=[("HE", hb, m)])

            def moe_down(ex, t, hb):
                wb = ex % 2
                for tb in range(4):
                    blk = t * 4 + tb
                    for half in range(2):
                        pd = 4 + (tb * 2 + half) % 4
                        mm_group(PS[pd][:, :], [(HE[hb][:, mc, tb * 128:(tb + 1) * 128], W2[wb][:, mc, half * 512:(half + 1) * 512]) for mc in range(4)],
                                 reads=[("HE", hb, m) for m in range(4)] + [("W2", wb)], writes=pk(pd))
                        p.add("dve", lambda e, pd=pd, blk=blk, half=half: e.scalar_tensor_tensor(
                            out=X[:, blk, half * 512:(half + 1) * 512], in0=PS[pd][:, :], scalar=GATES[:, blk, ex:ex + 1],
                            in1=X[:, blk, half * 512:(half + 1) * 512], op0=ALU.mult, op1=ALU.add),
                            reads=pk(pd) + ["GATES"] + xk(blk, half), writes=xk(blk, half))

            iters = [(ex, t) for ex in range(NE) for t in range(4)]
            for q in range(len(iters) + 1):
                if q < len(iters):
                    moe_gu(iters[q][0], iters[q][1], q % 2)
                if q >= 1:
                    moe_down(iters[q - 1][0], iters[q - 1][1], (q - 1) % 2)
            if stop_after == ("layer", li):
                break
        p.barrier()
        ar.reset()
        T1f = [ar.alloc(128, D, F32) for _ in range(4)]
        if do_final and stop_after is None:
            load_bc(0, final_g.partition_broadcast(128), [])
            rms_stats([(X[:, b, :], xk(b)) for b in range(16)], 1.0 / D)
            for blk in range(16):
                r = blk % 4
                p.add("dve", lambda e, r=r, blk=blk: e.scalar_tensor_tensor(
                    out=T1f[r], in0=X[:, blk, :], scalar=RSTD[:, blk:blk + 1], in1=BC[0][:], op0=ALU.mult, op1=ALU.mult),
                    reads=xk(blk) + ["RSTD", ("BC", 0)], writes=[("T1", r)])
                dma("sp", out4[s, blk * 128:(blk + 1) * 128, :], T1f[r], [("T1", r)], [("OUT", s, blk)], f"o{r}")
        else:
            for g4 in range(4):
                dma("sp", out4[s, g4 * 512:(g4 + 1) * 512, :].rearrange("(b p) d -> p b d", p=128), X[:, g4 * 4:(g4 + 1) * 4, :],
                    [k for b in range(g4 * 4, g4 * 4 + 4) for k in xk(b)], [("OUT", s, g4)], "od", 4)
    p.barrier()
    p.finalize_and_emit()
    st.close()
    return nc, p


import os
DBG_A = int(os.environ.get('K_DBG_A', 9))
DBG_B = int(os.environ.get('K_DBG_B', 9))
_CACHE = {}


def _run(inputs, layers, do_final, nseq=SEQ_PER_CORE, stop_after=None, x_override=None, ncores=NCORES):
    key = (tuple(layers), do_final, nseq, stop_after)
    if key not in _CACHE:
        _CACHE[key] = build_program(layers, do_final, nseq, stop_after)
    nc, _ = _CACHE[key]
    cst = _consts()
    f = lambda a: np.ascontiguousarray(np.asarray(a, dtype=np.float32))
    x = f(inputs["x"]) if x_override is None else x_override
    c = f(inputs["c"])
    ctx = f(inputs["ctx"])
    c_ctx = f(inputs["c_ctx"])
    shared = dict(
        norm1_g=f(inputs["norm1_g"]), norm2_g=f(inputs["norm2_g"]), w_mod=f(inputs["w_mod"]), b_mod=f(inputs["b_mod"]),
        gla_w_in=f(inputs["gla_w_in"])[0], gla_w_gate_a=f(inputs["gla_w_gate_a"])[0], gla_w_gate_b=f(inputs["gla_w_gate_b"])[0],
        gla_b_gate=f(inputs["gla_b_gate"])[0], gla_norm_g=f(inputs["gla_norm_g"]), gla_w_out=f(inputs["gla_w_out"])[0],
        pool_w=f(inputs["pool_w"])[0], pool_b=f(inputs["pool_b"]).reshape(1, D), pool_scale=f(inputs["pool_scale"]).reshape(1, D),
        w_router=f(inputs["w_router"]), b_router=f(inputs["b_router"]).reshape(1, NE),
        w_gate_e=f(inputs["w_gate_e"]), w_up_e=f(inputs["w_up_e"]), w_down_e=f(inputs["w_down_e"]),
        final_g=f(inputs["final_g"]).reshape(1, D), **cst)
    in_maps = []
    for k in range(ncores):
        b0 = k * nseq
        m = dict(shared)
        m["x4"] = np.ascontiguousarray(x[b0:b0 + nseq])
        m["ctx4"] = np.ascontiguousarray(ctx[b0:b0 + nseq])
        cc = np.concatenate([c[b0:b0 + nseq], np.zeros((4 - nseq, D), np.float32), c_ctx[None, :]], axis=0)
        m["ccT"] = np.ascontiguousarray(cc.T)
        in_maps.append(m)
    res = run_bass_kernel_spmd(nc, in_maps, core_ids=list(range(ncores)))
    return np.concatenate([np.asarray(r["out4"]) for r in res.results], axis=0)


def kernel(**inputs):
    return _run(inputs, (0, 1), True).astype(np.float32)
```

```python
import contextlib
import os
import numpy as np
import ml_dtypes
import concourse.bass as bass
import concourse.mybir as mybir
from concourse.bass_utils import run_bass_kernel_spmd

F32 = mybir.dt.float32
BF16 = mybir.dt.bfloat16
AF = mybir.ActivationFunctionType
ALU = mybir.AluOpType
AX = mybir.AxisListType

SIG_R = 4000
DMA_R = 250
EPS = 1e-6
NCORES = 8
SEQ_PER_CORE = 4
T = 2048
D = 1024
CTX = 256
NE = 16
DE = 512


class Prog:
    ENGS = ("pe", "act", "dve", "pool", "sp")

    def __init__(self, nc):
        self.nc = nc
        self.ops = []
        self.keys = {}
        self.eng_ops = {e: [] for e in self.ENGS}
        self.dma_cnt = {}
        self.last_dma = {}
        self.nobar = set()

    def add(self, eng, fn, reads=(), writes=(), dma=None, extra_deps=None):
        idx = len(self.ops)
        deps = {}
        psk = {("PS", k[1]) for k in list(reads) + list(writes) if isinstance(k, tuple) and k[0] == "PS"}
        if psk:
            reads = [k for k in reads if not (isinstance(k, tuple) and k[0] == "PS")]
            writes = [k for k in writes if not (isinstance(k, tuple) and k[0] == "PS")] + sorted(psk)
        for k in reads:
            st = self.keys.get(k)
            if st is not None and st[0] is not None:
                deps.setdefault(st[0], "raw")
        for k in writes:
            st = self.keys.get(k)
            if st is not None:
                if st[0] is not None and st[0] not in deps:
                    deps[st[0]] = "waw"
                for r in st[1]:
                    if r not in deps:
                        deps[r] = "war"
        if extra_deps:
            for d in extra_deps:
                deps[d] = "raw"
        if dma is not None and dma in self.last_dma:
            deps[self.last_dma[dma]] = "raw"
        for k in reads:
            st = self.keys.setdefault(k, [None, []])
            if dma is None:
                st[1] = [r for r in st[1] if not (self.ops[r]["dma"] is None and self.ops[r]["eng"] == eng)]
            st[1].append(idx)
        for k in writes:
            self.keys[k] = [idx, []]
        dcount = None
        if dma is not None:
            dcount = self.dma_cnt.get(dma, 0)
            self.dma_cnt[dma] = dcount + 1
            self.last_dma[dma] = idx
        op = dict(eng=eng, fn=fn, deps=deps, dma=dma, dcount=dcount, seq=len(self.eng_ops[eng]),
                  waits=[], signal=False)
        self.ops.append(op)
        self.eng_ops[eng].append(idx)
        return idx

    def barrier(self):
        last = {}
        for e in self.ENGS:
            for i in reversed(self.eng_ops[e]):
                if self.ops[i]["dma"] is None and self.ops[i]["fn"] is not None:
                    last[e] = i
                    break
        dl = [i for (nm, i) in self.last_dma.items() if nm not in self.nobar]
        for e in self.ENGS:
            deps = [i for (d, i) in last.items() if d != e] + dl
            self.add(e, None, extra_deps=deps)

    def finalize_and_emit(self):
        nc = self.nc
        ops = self.ops
        waited_seq = {e: {d: -1 for d in self.ENGS} for e in self.ENGS}
        waited_dma = {e: {} for e in self.ENGS}
        for i, op in enumerate(ops):
            E = op["eng"]
            for d, typ in op["deps"].items():
                dop = ops[d]
                if dop["dma"] is not None:
                    key = (dop["dma"], dop["dcount"] // DMA_R)
                    val = dop["dcount"] % DMA_R + 1
                    if waited_dma[E].get(key, 0) >= val:
                        continue
                    waited_dma[E][key] = val
                    op["waits"].append(("dma", key, val * 16))
                else:
                    Dn = dop["eng"]
                    if Dn == E:
                        if E == "pe" or typ != "raw":
                            continue
                    if waited_seq[E][Dn] >= dop["seq"]:
                        continue
                    waited_seq[E][Dn] = dop["seq"]
                    dop["signal"] = True
                    op["waits"].append(("cmp", d))
        sig_idx = {}
        nsig = {e: 0 for e in self.ENGS}
        for e in self.ENGS:
            for i in self.eng_ops[e]:
                if ops[i]["signal"]:
                    sig_idx[i] = nsig[e]
                    nsig[e] += 1
        stack = contextlib.ExitStack()
        sems = {}
        for e in self.ENGS:
            for g in range((nsig[e] + SIG_R - 1) // SIG_R):
                sems[("cmp", e, g)] = stack.enter_context(nc.semaphore(f"s_{e}_{g}"))
        for name, cnt in self.dma_cnt.items():
            for g in range((cnt + DMA_R - 1) // DMA_R):
                sems[("dma", name, g)] = stack.enter_context(nc.semaphore(f"d_{name}_{g}"))
        self.n_sems = len(sems)
        engmap = {"pe": "tensor", "act": "scalar", "dve": "vector", "pool": "gpsimd", "sp": "sync"}

        def emit_engine(e, eng):
            for i in self.eng_ops[e]:
                op = ops[i]
                for w in op["waits"]:
                    if w[0] == "dma":
                        _, key, val = w
                        eng.wait_ge(sems[("dma", key[0], key[1])], val)
                    else:
                        d = w[1]
                        si = sig_idx[d]
                        eng.wait_ge(sems[("cmp", ops[d]["eng"], si // SIG_R)], si % SIG_R + 1)
                if op["fn"] is None:
                    continue
                inst = op["fn"](eng)
                if inst is None:
                    continue
                if op["dma"] is not None:
                    inst.then_inc(sems[("dma", op["dma"], op["dcount"] // DMA_R)], 16)
                elif op["signal"]:
                    si = sig_idx[i]
                    inst.then_inc(sems[("cmp", e, si // SIG_R)], 1)

        with stack:
            with nc.Block() as block:
                for e in self.ENGS:
                    if not self.eng_ops[e]:
                        continue

                    def _mk(e=e):
                        def _f(eng):
                            emit_engine(e, eng)
                        return _f
                    getattr(block, engmap[e])(_mk())


def _consts():
    c = {}
    c["ident_f"] = np.eye(128, dtype=np.float32)
    c["ident_b"] = np.eye(128, dtype=np.float32).astype(ml_dtypes.bfloat16)
    j = np.arange(128)[:, None]
    i = np.arange(128)[None, :]
    tri = np.zeros((128, 4, 128), np.float32)
    tri[:, 0, :] = (j <= i) * (-1.0 / 16)
    tri[:, 1, :] = (j >= i) * (-1.0 / 16)
    tri[:, 2, :] = (j > i) * (-1.0 / 16)
    tri[:, 3, :] = (j < i) * (-1.0 / 16)
    c["tri"] = tri
    msk = np.zeros((128, 2, 128), np.float32)
    msk[:, 0, :] = (j <= i)
    msk[:, 1, :] = (j >= i)
    c["msk"] = msk
    pm = np.zeros((128, 4, 128), np.float32)
    for gi, w in enumerate((2, 4, 8, 16)):
        for blk in range(2):
            for pos in range(64):
                lo = min(max(pos - w // 2, 0), 64)
                hi = min(max(pos - w // 2 + w, 0), 64)
                cnt = hi - lo
                for jj in range(lo, hi):
                    pm[blk * 64 + jj, gi, blk * 64 + pos] += 1.0 / cnt
                pm[blk * 64 + pos, gi, blk * 64 + pos] -= 1.0
    c["poolm"] = pm.astype(ml_dtypes.bfloat16)
    c["ones_b"] = np.ones((1, 128), np.float32).astype(ml_dtypes.bfloat16)
    return c


DBG_OFFS = {}


class Arena:
    def __init__(self, ten, nbytes):
        self.ten = ten
        self.nbytes = nbytes
        self.off = 0
        self.gen = 0

    def reset(self):
        self.off = 0
        self.gen += 1

    def alloc(self, parts, nelem, dt, name=None):
        if name:
            DBG_OFFS[name] = (self.off, nelem, dt == F32)
        esz = 4 if dt == F32 else 2
        nb = (nelem * esz + 63) // 64 * 64
        assert self.off + nb <= self.nbytes, (self.off, nb, self.nbytes)
        a = self.ten[0:parts, self.off // 2:(self.off + nelem * esz) // 2]
        self.off += nb
        if dt == F32:
            a = a.bitcast(F32)
        return a


def build_program(layers=(0, 1), do_final=True, nseq=SEQ_PER_CORE, stop_after=None):
    nc = bass.Bass("TRN2", target_bir_lowering=False)

    def din(name, shape, dt=F32):
        return nc.dram_tensor(name, list(shape), dt, kind="ExternalInput").ap()

    def dint(name, shape, dt):
        return nc.dram_tensor(name, list(shape), dt, kind="Internal").ap()

    x4 = din("x4", [nseq, T, D])
    ctx4 = din("ctx4", [nseq, CTX, D])
    ccT = din("ccT", [D, 5])
    norm1_g = din("norm1_g", [2, D])
    norm2_g = din("norm2_g", [2, D])
    w_mod = din("w_mod", [2, D, 6 * D])
    b_mod = din("b_mod", [2, 6 * D])
    w_in = din("gla_w_in", [D, 3072])
    w_ga = din("gla_w_gate_a", [2, D, 16])
    w_gb = din("gla_w_gate_b", [2, 16, 512])
    b_g = din("gla_b_gate", [2, 512])
    gnorm = din("gla_norm_g", [1, 256])
    w_out = din("gla_w_out", [D, D])
    pool_w = din("pool_w", [4, 256, 256])
    pool_b = din("pool_b", [1, D])
    pool_scale = din("pool_scale", [1, D])
    w_router = din("w_router", [D, NE])
    b_router = din("b_router", [1, NE])
    w_gate_e = din("w_gate_e", [2, NE, D, DE])
    w_up_e = din("w_up_e", [2, NE, D, DE])
    w_down_e = din("w_down_e", [2, NE, DE, D])
    final_g = din("final_g", [1, D])
    ident_f = din("ident_f", [128, 128])
    ident_b = din("ident_b", [128, 128], BF16)
    tri_d = din("tri", [128, 4, 128])
    msk_d = din("msk", [128, 2, 128])
    poolm_d = din("poolm", [128, 4, 128], BF16)
    ones_d = din("ones_b", [1, 128], BF16)
    out4 = nc.dram_tensor("out4", [nseq, T, D], F32, kind="ExternalOutput").ap()

    WG = dint("WGs", [2, NE, D, DE], BF16)
    WU = dint("WUs", [2, NE, D, DE], BF16)
    WD = dint("WDs", [2, NE, DE, D], BF16)
    WIN = dint("WINs", [D, 3072], BF16)
    WOUT = dint("WOUTs", [D, D], BF16)
    WPOOL = dint("WPOOLs", [4, 256, 256], BF16)
    WGAs = dint("WGAs", [D, 32], BF16)
    MODS = dint("MODS", [2, 5, 6 * D], F32)

    st = contextlib.ExitStack()

    def sb(name, shape, dt):
        return st.enter_context(nc.sbuf_tensor(name, list(shape), dt))

    X = sb("X", [128, 16, D], F32)
    HT = sb("HT", [128, 8, T], BF16)
    ARENA_BYTES = 70 * 1024
    ARENA_T = sb("ARENA", [128, ARENA_BYTES // 2], BF16)
    ar = Arena(ARENA_T, ARENA_BYTES)
    BC = [sb(f"BC{i}", [128, D], F32) for i in range(3)]
    IDF = sb("IDF", [128, 128], F32)
    IDB = sb("IDB", [128, 128], BF16)
    TRI = sb("TRI", [128, 4, 128], F32)
    MSK = sb("MSK", [128, 2, 128], F32)
    POOLM = sb("POOLM", [128, 4, 128], BF16)
    ONES = sb("ONES", [1, 128], BF16)
    PBR = sb("PBR", [1, D], BF16)
    WR = sb("WR", [128, 8, NE], F32)
    BRB = sb("BRB", [128, 16, NE], F32)
    WGA = sb("WGA", [128, 8, 32], BF16)
    WGB = sb("WGB", [64, D], BF16)
    NGB = sb("NGB", [128, 256], F32)
    WP = sb("WP", [128, 4, 2, 256], BF16)
    SSQ = sb("SSQ", [128, 18], F32)
    SQT = sb("SQT", [128, 18], F32)
    RSTD = sb("RSTD", [128, 18], F32)
    SC = sb("SC", [128, 16, NE], F32)
    GATES = sb("GATES", [128, 16, NE], F32)
    RT = [sb(f"RT{i}", [128, 16, NE], F32) for i in range(4)]
    RS = [sb(f"RS{i}", [128, 64], F32) for i in range(4)]
    PS = [st.enter_context(nc.psum_tensor(f"PS{i}", [128, 512], F32)) for i in range(8)]

    p = Prog(nc)
    uid = [0]

    def U():
        uid[0] += 1
        return uid[0]

    rot = {}

    def dma(eng, out, in_, reads, writes, name, nrot=1):
        base = name
        if nrot > 1:
            k = rot.get(name, 0)
            rot[name] = k + 1
            name = f"{name}_{k % nrot}"
        if base == "cast":
            p.nobar.add(name)
        p.add(eng, lambda e: e.dma_start(out=out, in_=in_), reads=reads, writes=writes, dma=name)

    def pk(i, lo=0, hi=512):
        return [("PS", i, c) for c in range(lo // 128, (hi + 127) // 128)]

    def mm_group(out, pairs, reads, writes, f32=False):
        n = len(pairs)

        def fn(e):
            inst = None
            for q, (l, r) in enumerate(pairs):
                inst = e.matmul(out, lhsT=l, rhs=r, start=(q == 0), stop=(q == n - 1))
            return inst
        p.add("pe", fn, reads=reads, writes=writes)

    dma("sp", IDF[:], ident_f, [], ["IDF"], "c0", 8)
    dma("sp", IDB[:], ident_b, [], ["IDB"], "c0", 8)
    dma("sp", TRI[:], tri_d, [], ["TRI"], "c0", 8)
    dma("sp", MSK[:], msk_d, [], ["MSK"], "c0", 8)
    dma("sp", POOLM[:], poolm_d, [], ["POOLM"], "c0", 8)
    dma("sp", ONES[:], ones_d, [], ["ONES"], "c0", 8)
    dma("sp", WR[:], w_router.rearrange("(kc p) n -> p kc n", p=128), [], ["WR"], "c0", 8)
    dma("sp", NGB[:], gnorm.partition_broadcast(128), [], ["NGB"], "c0", 8)
    dma("sp", BRB[:, 0, :], b_router.partition_broadcast(128), [], ["BRB0"], "c0", 8)
    for b in range(1, 16):
        p.add("dve", lambda e, b=b: e.tensor_copy(out=BRB[:, b, :], in_=BRB[:, 0, :]), reads=["BRB0"], writes=[("BRB", b)])
    brb_keys = ["BRB0"] + [("BRB", b) for b in range(1, 16)]
    if 0 in layers:
        dma("pool", WIN, w_in, [], ["WIN"], "cast", 12)
        dma("pool", WOUT, w_out, [], ["WOUT"], "cast", 12)
        for z in range(2):
            dma("pool", WGAs[:, z * 16:(z + 1) * 16], w_ga[z], [], [("WGAs", z)], "cast", 12)
        p.add("pool", lambda e: e.memset(WGB[:], 0.0), writes=["WGB"])
        for z in range(2):
            dma("pool", WGB[z * 16:(z + 1) * 16, z * 512:(z + 1) * 512], w_gb[z], ["WGB"], [("WGBp", z)], "cast", 12)
            dma("pool", WGB[32:33, z * 512:(z + 1) * 512], b_g[z:z + 1, :], ["WGB"], [("WGBb", z)], "cast", 12)
        wgb_keys = ["WGB"] + [("WGBp", z) for z in range(2)] + [("WGBb", z) for z in range(2)]
        dma("sp", WGA[:], WGAs.rearrange("(kc p) n -> p kc n", p=128), [("WGAs", 0), ("WGAs", 1)], ["WGA"], "c0", 8)
    if 1 in layers:
        dma("pool", WPOOL, pool_w, [], ["WPOOL"], "cast", 12)
        dma("pool", PBR[:], pool_b, [], ["PBR"], "cast", 12)
        dma("sp", WP[:], WPOOL.rearrange("g (kc p) n -> p g kc n", p=128), ["WPOOL"], ["WP"], "c0", 8)
    pending_casts = []
    defer_l1 = (tuple(layers) == (0, 1)) and stop_after is None and os.environ.get("K_DBG_NOEXP") != "1"

    def cast_one(li, e_, which):
        if which == 0:
            dma("pool", WG[li, e_], w_gate_e[li, e_], [], [("WG", li, e_)], "cast", 12)
        elif which == 1:
            dma("pool", WU[li, e_], w_up_e[li, e_], [], [("WU", li, e_)], "cast", 12)
        else:
            dma("pool", WD[li, e_], w_down_e[li, e_], [], [("WD", li, e_)], "cast", 12)

    for li in layers:
        if os.environ.get("K_DBG_NOEXP") == "1":
            break
        for e_ in range(NE):
            for which in range(3):
                if li == 1 and defer_l1:
                    pending_casts.append((li, e_, which))
                else:
                    cast_one(li, e_, which)
    scan_step_ctr = [0]

    ar.reset()
    CC = ar.alloc(128, 8 * 5, F32).rearrange("p (a b) -> p a b", a=8)
    SCC = ar.alloc(128, 8 * 5, F32).rearrange("p (a b) -> p a b", a=8)
    WM = [ar.alloc(128, 8 * 512, F32).rearrange("p (a b) -> p a b", a=8) for _ in range(2)]
    BMT = [ar.alloc(5, 512, F32) for _ in range(2)]
    NGT = [ar.alloc(5, 512, F32) for _ in range(2)]
    MT = [ar.alloc(5, 512, F32) for _ in range(2)]
    dma("sp", CC, ccT.rearrange("(kc p) n -> p kc n", p=128), [], ["CC"], "c0", 8)
    p.add("act", lambda e: e.activation(out=SCC, in_=CC, func=AF.Silu), reads=["CC"], writes=["SCC"])
    q = 0
    for li in layers:
        for ct in range(12):
            b = q % 2
            q += 1
            dma("sp", WM[b], w_mod[li][:, ct * 512:(ct + 1) * 512].rearrange("(kc p) n -> p kc n", p=128), [], [("WM", b)], f"wm{b}", 3)
            dma("sp", BMT[b], b_mod[li:li + 1, ct * 512:(ct + 1) * 512].partition_broadcast(5), [], [("BMT", b)], f"wm{b}", 3)
            is_sc = ct in (2, 3, 8, 9)
            if is_sc:
                ng = norm1_g if ct < 6 else norm2_g
                dma("sp", NGT[b], ng[li:li + 1, (ct % 2) * 512:(ct % 2 + 1) * 512].partition_broadcast(5), [], [("NGT", b)], f"wm{b}", 3)
            mm_group(PS[b][0:5, :], [(SCC[:, kc, :], WM[b][:, kc, :]) for kc in range(8)],
                     reads=["SCC", ("WM", b)], writes=pk(b))
            p.add("dve", lambda e, b=b: e.tensor_tensor(out=MT[b], in0=PS[b][0:5, :], in1=BMT[b], op=ALU.add),
                  reads=pk(b) + [("BMT", b)], writes=[("MT", b)])
            if is_sc:
                p.add("dve", lambda e, b=b: e.scalar_tensor_tensor(out=MT[b], in0=MT[b], scalar=1.0, in1=NGT[b], op0=ALU.add, op1=ALU.mult),
                      reads=[("MT", b), ("NGT", b)], writes=[("MT", b)])
            dma("sp", MODS[li, :, ct * 512:(ct + 1) * 512], MT[b], [("MT", b)], [("MODS", li, ct)], f"mt{b}")

    def mods_row(li, row, part):
        return MODS[li, row:row + 1, part * D:(part + 1) * D].partition_broadcast(128)

    def mods_keys(li, part):
        return [("MODS", li, 2 * part), ("MODS", li, 2 * part + 1)]

    def load_bc(i, src, reads):
        dma("sp", BC[i][:], src, reads, [("BC", i)], f"bc{i}")

    def rms_stats(srcs, scale):
        n = len(srcs)
        junk = ar.alloc(128, D, F32)
        ju = U()
        for i, (a, rk) in enumerate(srcs):
            w = a.shape[-1]
            p.add("act", lambda e, a=a, i=i, w=w: e.activation(out=junk[:, 0:w], in_=a, func=AF.Square, accum_out=SSQ[:, i:i + 1]),
                  reads=rk, writes=[("junk", ju), ("SSQ", i)])
        p.add("act", lambda e: e.activation(out=SQT[:, 0:n], in_=SSQ[:, 0:n], func=AF.Sqrt, bias=EPS, scale=scale),
              reads=[("SSQ", i) for i in range(n)], writes=["SQT"])
        p.add("dve", lambda e: e.reciprocal(out=RSTD[:, 0:n], in_=SQT[:, 0:n]), reads=["SQT"], writes=["RSTD"])

    def xk(blk, half=None):
        if half is None:
            return [("X", blk, 0), ("X", blk, 1)]
        return [("X", blk, half)]

    for s in range(nseq):
        if stop_after == "pro":
            break
        for g4 in range(4):
            dma("sp", X[:, g4 * 4:(g4 + 1) * 4, :], x4[s, g4 * 512:(g4 + 1) * 512, :].rearrange("(b p) d -> p b d", p=128),
                [], [k for b in range(g4 * 4, g4 * 4 + 4) for k in xk(b)], "x", 4)
        for li in layers:
            p.barrier()
            ar.reset()
            if li == 0:
                HCT = ar.alloc(128, 8 * CTX, BF16, name='HCT').rearrange("p (a b) -> p a b", a=8)
                mk = ar.off
                CX = ar.alloc(128, 2 * D, F32, name='CX').rearrange("p (a b) -> p a b", a=2)
                T1 = [ar.alloc(128, D, F32, name=f'T1_{_i}') for _i in range(2)]
                HB = [ar.alloc(128, D, BF16, name=f'HB_{_i}') for _i in range(2)]
                dma("sp", CX, ctx4[s].rearrange("(b p) d -> p b d", p=128), [], ["CX"], "cx")
                load_bc(0, mods_row(0, s, 1), mods_keys(0, 1))
                load_bc(1, mods_row(0, s, 0), mods_keys(0, 0))
                rms_stats([(X[:, b, :], xk(b)) for b in range(16)] + [(CX[:, b, :], ["CX"]) for b in range(2)], 1.0 / D)
                def n1a(blk):
                    if blk == 16:
                        load_bc(0, mods_row(0, 4, 1), mods_keys(0, 1))
                        load_bc(1, mods_row(0, 4, 0), mods_keys(0, 0))
                    b2 = blk % 2
                    src = X[:, blk, :] if blk < 16 else CX[:, blk - 16, :]
                    srck = xk(blk) if blk < 16 else ["CX"]
                    p.add("dve", lambda e: e.scalar_tensor_tensor(
                        out=T1[b2], in0=src, scalar=RSTD[:, blk:blk + 1], in1=BC[0][:], op0=ALU.mult, op1=ALU.mult),
                        reads=srck + ["RSTD", ("BC", 0)], writes=[("T1", b2)])
                    p.add("pool", lambda e: e.tensor_tensor(out=HB[b2], in0=T1[b2], in1=BC[1][:], op=ALU.add),
                          reads=[("T1", b2), ("BC", 1)], writes=[("HB", b2)])

                def n1b(blk):
                    b2 = blk % 2
                    for hh in range(2):
                        pb = b2 * 2 + hh
                        for kc4 in range(4):
                            kc = hh * 4 + kc4
                            mm_group(PS[pb][:, kc4 * 128:(kc4 + 1) * 128], [(HB[b2][:, kc * 128:(kc + 1) * 128], IDB[:])],
                                     reads=[("HB", b2), "IDB"], writes=pk(pb, kc4 * 128, kc4 * 128 + 128))
                        if blk < 16:
                            dst = HT[:, hh * 4:(hh + 1) * 4, blk * 128:(blk + 1) * 128]
                            dk = [("HT", blk)]
                        else:
                            dst = HCT[:, hh * 4:(hh + 1) * 4, (blk - 16) * 128:(blk - 15) * 128]
                            dk = [("HCT", blk - 16)]
                        p.add("act", lambda e, dst=dst, pb=pb: e.activation(out=dst, in_=PS[pb][:].rearrange("p (a b) -> p a b", a=4), func=AF.Copy),
                              reads=pk(pb), writes=dk)

                for it in range(19):
                    if it < 18:
                        n1a(it)
                    if it >= 1:
                        n1b(it - 1)
                if stop_after == "n1":
                    break
                p.barrier()
                ar.off = mk
                load_bc(2, mods_row(0, s, 2), mods_keys(0, 2))
                LOWT = ar.alloc(64, 2304, BF16)
                WINH = ar.alloc(128, 8 * 768, BF16).rearrange("p (a b) -> p a b", a=8)
                WOUTH = ar.alloc(128, 2 * D, BF16).rearrange("p (a b) -> p a b", a=2)
                OBUF = ar.alloc(128, 16 * 256, F32).rearrange("p (a b) -> p a b", a=16)
                E1 = [ar.alloc(128, 128, F32) for _ in range(2)]
                SP_ = [ar.alloc(128, 128, F32, name=f'SP{_i}') for _i in range(2)]
                EQR = [ar.alloc(128, 256, F32, name=f'EQR{_i}') for _i in range(3)]
                EK = [ar.alloc(128, 128, F32) for _ in range(2)]
                QT = [ar.alloc(128, 128, BF16) for _ in range(3)]
                KT = [ar.alloc(128, 128, BF16) for _ in range(3)]
                KH = [ar.alloc(128, 128, BF16) for _ in range(3)]
                VB = [ar.alloc(128, 256, BF16) for _ in range(3)]
                ATM = [ar.alloc(128, 128, BF16) for _ in range(2)]
                SS = [ar.alloc(128, 256, F32, name=f'SS{_i}') for _i in range(2)]
                SBF = [[ar.alloc(128, 256, BF16) for _ in range(2)] for _ in range(2)]
                SR = [ar.alloc(128, 256, F32) for _ in range(2)]
                TT = [ar.alloc(128, 256, F32) for _ in range(2)]
                OGB = [ar.alloc(128, 256, BF16) for _ in range(2)]
                OGT2 = [ar.alloc(128, 256, BF16).rearrange("p (a b) -> p a b", a=2) for _ in range(2)]
                TMP = [ar.alloc(128, 512, F32) for _ in range(4)]

                def hT(c, kc):
                    if c < 16:
                        return HT[:, kc, c * 128:(c + 1) * 128], ("HT", c)
                    return HCT[:, kc, (c - 16) * 128:(c - 15) * 128], ("HCT", c - 16)

                p.add("pool", lambda e: e.memset(LOWT, 1.0), writes=["LOWTm"])
                for tt in range(5):
                    if tt < 4:
                        rh = [HT[:, kc, tt * 512:(tt + 1) * 512] for kc in range(8)]
                        rk = [("HT", b) for b in range(tt * 4, tt * 4 + 4)]
                        w = 512
                    else:
                        rh = [HCT[:, kc, :] for kc in range(8)]
                        rk = [("HCT", 0), ("HCT", 1)]
                        w = 256
                    pb = tt % 2
                    mm_group(PS[pb][0:32, 0:w], [(WGA[:, kc, :], rh[kc]) for kc in range(8)], reads=rk + ["WGA"], writes=pk(pb, 0, w))
                    p.add("act", lambda e, tt=tt, w=w, pb=pb: e.activation(out=LOWT[0:32, tt * 512:tt * 512 + w], in_=PS[pb][0:32, 0:w], func=AF.Copy),
                          reads=pk(pb, 0, w) + ["LOWTm"], writes=[("LOWT", tt)])

                if stop_after == "low":
                    break
                stopped = False
                for h in range(4):
                    if stop_after == ("head", h):
                        stopped = True
                        break
                    hu = U()
                    dma("sp", WINH[:, :, 0:128], WIN[:, h * 128:(h + 1) * 128].rearrange("(kc p) n -> p kc n", p=128), ["WIN"], [("WINH", 0)], "winh", 5)
                    dma("sp", WINH[:, :, 128:256], WIN[:, 512 + h * 128:512 + (h + 1) * 128].rearrange("(kc p) n -> p kc n", p=128), ["WIN"], [("WINH", 1)], "winh", 5)
                    dma("sp", WINH[:, :, 256:512], WIN[:, 1024 + h * 256:1024 + (h + 1) * 256].rearrange("(kc p) n -> p kc n", p=128), ["WIN"], [("WINH", 2)], "winh", 5)
                    dma("sp", WINH[:, :, 512:768], WIN[:, 2048 + h * 256:2048 + (h + 1) * 256].rearrange("(kc p) n -> p kc n", p=128), ["WIN"], [("WINH", 3)], "winh", 5)
                    dma("sp", WOUTH, WOUT[h * 256:(h + 1) * 256, :].rearrange("(kc p) n -> p kc n", p=128), ["WOUT"], ["WOUTH"], "winh", 5)
                    for dr in range(2):
                        p.add("pool", lambda e, dr=dr: e.memset(SS[dr], 0.0), writes=[("SS", dr)])
                        p.add("pool", lambda e, dr=dr: e.memset(SBF[dr][0], 0.0), writes=[("SBF", dr, 0)])
                    order = [[16, 17] + list(range(16)), [17, 16] + list(range(15, -1, -1))]
                    steps = []
                    for i in range(18):
                        steps.append((0, order[0][i], i))
                        steps.append((1, order[1][i], i))

                    def S1(k, dr, c, i):
                        r2 = k % 2
                        A0, A1, ZB = 3 * r2, 3 * r2 + 1, 3 * r2 + 2
                        hts = [hT(c, kc) for kc in range(8)]
                        hk = [hts[0][1]]
                        zc = dr * 512 + h * 128
                        mm_group(PS[ZB][:, 0:128], [(LOWT[0:64, c * 128:(c + 1) * 128], WGB[0:64, zc:zc + 128])],
                                 reads=[("LOWT", c // 4), "LOWTm"] + wgb_keys, writes=pk(ZB))
                        p.add("act", lambda e: e.activation(out=E1[r2], in_=PS[ZB][:, 0:128], func=AF.Exp, scale=-1.0),
                              reads=pk(ZB), writes=[("E1", r2)])
                        p.add("act", lambda e: e.activation(out=SP_[r2], in_=E1[r2], func=AF.Ln, bias=1.0),
                              reads=[("E1", r2)], writes=[("SP", r2)])
                        mm_group(PS[A0][:, 0:384], [(hts[kc][0], WINH[:, kc, 128:512]) for kc in range(8)],
                                 reads=hk + [("WINH", 1), ("WINH", 2)], writes=pk(A0))
                        mm_group(PS[A0][:, 384:512], [(WINH[:, kc, 128:256], hts[kc][0]) for kc in range(8)],
                                 reads=hk + [("WINH", 1)], writes=pk(A0))
                        mm_group(PS[A1][:, 0:128], [(WINH[:, kc, 0:128], hts[kc][0]) for kc in range(8)],
                                 reads=hk + [("WINH", 0)], writes=pk(A1))

                    def S2(k, dr, c, i):
                        r2 = k % 2
                        r3 = k % 3
                        A0, A1, ZB = 3 * r2, 3 * r2 + 1, 3 * r2 + 2
                        mm_group(PS[ZB][:, 128:256], [(SP_[r2], TRI[:, dr, :])], reads=[("SP", r2), "TRI"], writes=pk(ZB))
                        mm_group(PS[ZB][:, 256:384], [(TRI[:, 2 + dr, :], SP_[r2])], reads=[("SP", r2), "TRI"], writes=pk(ZB))
                        p.add("act", lambda e: e.activation(out=EQR[r3], in_=PS[ZB][:, 128:384], func=AF.Exp),
                              reads=pk(ZB), writes=[("EQR", r3)])
                        p.add("act", lambda e: e.activation(out=EK[r2], in_=PS[ZB][:, 128:256], func=AF.Exp, scale=-1.0),
                              reads=pk(ZB), writes=[("EK", r2)])
                        p.add("act", lambda e: e.activation(out=VB[r3], in_=PS[A0][:, 128:384], func=AF.Copy),
                              reads=pk(A0), writes=[("VB", r3)])
                        p.add("dve", lambda e: e.scalar_tensor_tensor(out=QT[r3], in0=PS[A1][:, 0:128], scalar=float(128 ** -0.5), in1=EQR[r3][:, 0:128],
                                                                      op0=ALU.mult, op1=ALU.mult),
                              reads=pk(A1) + [("EQR", r3)], writes=[("QT", r3)])
                        p.add("dve", lambda e: e.tensor_tensor(out=KT[r3], in0=PS[A0][:, 384:512], in1=EK[r2], op=ALU.mult),
                              reads=pk(A0) + [("EK", r2)], writes=[("KT", r3)])
                        p.add("dve", lambda e: e.tensor_tensor(out=KH[r3], in0=PS[A0][:, 0:128], in1=EQR[r3][:, 128:256], op=ALU.mult),
                              reads=pk(A0) + [("EQR", r3)], writes=[("KH", r3)])

                    def S3(k, dr, c, i):
                        r2 = k % 2
                        r3 = k % 3
                        A1 = 3 * r2 + 1
                        mm_group(PS[A1][:, 128:256], [(KT[r3], QT[r3])], reads=[("KT", r3), ("QT", r3)], writes=pk(A1))
                        p.add("dve", lambda e: e.tensor_tensor(out=ATM[r2], in0=PS[A1][:, 128:256], in1=MSK[:, dr, :], op=ALU.mult),
                              reads=pk(A1) + ["MSK"], writes=[("ATM", r2)])

                    def S4(k, dr, c, i):
                        r2 = k % 2
                        r3 = k % 3
                        BO = 6 + dr
                        sb_cur = SBF[dr][i % 2]
                        sb_nxt = SBF[dr][(i + 1) % 2]
                        if c < 16:
                            mm_group(PS[BO][:, 0:256], [(ATM[r2], VB[r3]), (QT[r3], sb_cur)],
                                     reads=[("ATM", r2), ("VB", r3), ("QT", r3), ("SBF", dr, i % 2)], writes=pk(BO))
                        mm_group(PS[BO][:, 256:512], [(KH[r3], VB[r3])], reads=[("KH", r3), ("VB", r3)], writes=pk(BO))
                        if c < 16:
                            first = (dr == 0) if c <= 7 else (dr == 1)
                            if first:
                                p.add("act", lambda e: e.activation(out=OBUF[:, c, :], in_=PS[BO][:, 0:256], func=AF.Copy),
                                      reads=pk(BO), writes=[("OBUF", c)])
                            else:
                                p.add("dve", lambda e: e.tensor_tensor(out=OBUF[:, c, :], in0=PS[BO][:, 0:256], in1=OBUF[:, c, :], op=ALU.add),
                                      reads=pk(BO) + [("OBUF", c)], writes=[("OBUF", c)])
                        edge = 127 if dr == 0 else 0
                        p.add("dve", lambda e: e.scalar_tensor_tensor(out=SS[dr], in0=SS[dr], scalar=EQR[r3][:, edge:edge + 1], in1=PS[BO][:, 256:512],
                                                                      op0=ALU.mult, op1=ALU.add),
                              reads=[("SS", dr), ("EQR", r3)] + pk(BO), writes=[("SS", dr)])
                        p.add("pool", lambda e: e.tensor_copy(out=sb_nxt, in_=SS[dr]), reads=[("SS", dr)], writes=[("SBF", dr, (i + 1) % 2)])
                        scan_step_ctr[0] += 1
                        if pending_casts and scan_step_ctr[0] % 3 == 0:
                            cast_one(*pending_casts.pop(0))

                    nst = len(steps)
                    for it in range(nst + 3):
                        if it < nst:
                            S1(it, *steps[it])
                        if 0 <= it - 1 < nst:
                            S2(it - 1, *steps[it - 1])
                        if 0 <= it - 2 < nst:
                            S3(it - 2, *steps[it - 2])
                        if 0 <= it - 3 < nst:
                            S4(it - 3, *steps[it - 3])

                    if stop_after == ("scan", h):
                        stopped = True
                        break
                    junk = TT[0]
                    for c in range(16):
                        p.add("act", lambda e, c=c: e.activation(out=junk, in_=OBUF[:, c, :], func=AF.Square, accum_out=SSQ[:, c:c + 1]),
                              reads=[("OBUF", c)], writes=[("TT", 0), ("SSQ", c)])
                    p.add("act", lambda e: e.activation(out=SQT[:, 0:16], in_=SSQ[:, 0:16], func=AF.Sqrt, bias=EPS, scale=1.0 / 256),
                          reads=[("SSQ", i) for i in range(16)], writes=["SQT"])
                    p.add("dve", lambda e: e.reciprocal(out=RSTD[:, 0:16], in_=SQT[:, 0:16]), reads=["SQT"], writes=["RSTD"])
                    def ro1(c):
                        r = c % 2
                        b0 = 4 * r
                        hts = [hT(c, kc) for kc in range(8)]
                        mm_group(PS[b0][:, 0:256], [(hts[kc][0], WINH[:, kc, 512:768]) for kc in range(8)],
                                 reads=[hts[0][1], ("WINH", 3)], writes=pk(b0, 0, 256))
                        p.add("act", lambda e: e.activation(out=SR[r], in_=PS[b0][:, 0:256], func=AF.Silu),
                              reads=pk(b0, 0, 256), writes=[("SR", r)])
                        p.add("dve", lambda e: e.scalar_tensor_tensor(out=TT[r], in0=OBUF[:, c, :], scalar=RSTD[:, c:c + 1], in1=NGB[:],
                                                                      op0=ALU.mult, op1=ALU.mult),
                              reads=[("OBUF", c), "RSTD", "NGB"], writes=[("TT", r)])
                        p.add("pool", lambda e: e.tensor_tensor(out=OGB[r], in0=TT[r], in1=SR[r], op=ALU.mult),
                              reads=[("TT", r), ("SR", r)], writes=[("OGB", r)])

                    def ro2(c):
                        r = c % 2
                        b0 = 4 * r
                        for fc in range(2):
                            mm_group(PS[b0 + 3][:, fc * 128:(fc + 1) * 128], [(OGB[r][:, fc * 128:(fc + 1) * 128], IDB[:])],
                                     reads=[("OGB", r), "IDB"], writes=pk(b0 + 3))
                        p.add("act", lambda e: e.activation(out=OGT2[r], in_=PS[b0 + 3][:, 0:256].rearrange("p (a b) -> p a b", a=2), func=AF.Copy),
                              reads=pk(b0 + 3), writes=[("OGT2", r)])

                    def ro3(c):
                        r = c % 2
                        b0 = 4 * r
                        for half in range(2):
                            pb = b0 + 1 + half
                            ti = r * 2 + half
                            mm_group(PS[pb][:, :], [(OGT2[r][:, kc2, :], WOUTH[:, kc2, half * 512:(half + 1) * 512]) for kc2 in range(2)],
                                     reads=[("OGT2", r), "WOUTH"], writes=pk(pb))
                            p.add("dve", lambda e, pb=pb, ti=ti, half=half: e.tensor_tensor(out=TMP[ti], in0=PS[pb][:, :], in1=BC[2][:, half * 512:(half + 1) * 512], op=ALU.mult),
                                  reads=pk(pb) + [("BC", 2)], writes=[("TMP", ti)])
                            p.add("pool", lambda e, ti=ti, half=half: e.tensor_tensor(out=X[:, c, half * 512:(half + 1) * 512], in0=X[:, c, half * 512:(half + 1) * 512], in1=TMP[ti], op=ALU.add),
                                  reads=[("TMP", ti)] + xk(c, half), writes=xk(c, half))

                    for c in range(-1, 17):
                        if 0 <= c + 1 < 16:
                            ro1(c + 1)
                        if 0 <= c < 16:
                            ro2(c)
                        if 0 <= c - 1 < 16:
                            ro3(c - 1)
                if stopped:
                    break
            else:
                T1p = [ar.alloc(128, D, F32) for _ in range(2)]
                HBp = [ar.alloc(128, D, BF16) for _ in range(2)]
                PT = [ar.alloc(128, 8 * 128, BF16).rearrange("p (a b) -> p a b", a=8) for _ in range(2)]
                TMPY = [ar.alloc(128, D, F32) for _ in range(2)]
                load_bc(0, mods_row(1, s, 1), mods_keys(1, 1))
                load_bc(1, mods_row(1, s, 0), mods_keys(1, 0))
                load_bc(2, mods_row(1, s, 2), mods_keys(1, 2))
                dma("sp", TMPY[0], pool_scale.partition_broadcast(128), [], [("TMPY", 0)], "psc")
                p.add("dve", lambda e: e.tensor_tensor(out=BC[2][:], in0=BC[2][:], in1=TMPY[0], op=ALU.mult),
                      reads=[("BC", 2), ("TMPY", 0)], writes=[("BC", 2)])
                rms_stats([(X[:, b, :], xk(b)) for b in range(16)], 1.0 / D)
                def pma(blk):
                    r = blk % 2
                    p.add("dve", lambda e: e.scalar_tensor_tensor(
                        out=T1p[r], in0=X[:, blk, :], scalar=RSTD[:, blk:blk + 1], in1=BC[0][:], op0=ALU.mult, op1=ALU.mult),
                        reads=xk(blk) + ["RSTD", ("BC", 0)], writes=[("T1", r)])
                    p.add("pool", lambda e: e.tensor_tensor(out=HBp[r], in0=T1p[r], in1=BC[1][:], op=ALU.add),
                          reads=[("T1", r), ("BC", 1)], writes=[("HB", r)])

                def pmb(blk):
                    r = blk % 2
                    b0 = 4 * r
                    for hh in range(2):
                        pb = b0 + hh
                        for kc4 in range(4):
                            fc = hh * 4 + kc4
                            mm_group(PS[pb][:, kc4 * 128:(kc4 + 1) * 128], [(HBp[r][:, fc * 128:(fc + 1) * 128], POOLM[:, fc // 2, :])],
                                     reads=[("HB", r), "POOLM"], writes=pk(pb, kc4 * 128, kc4 * 128 + 128))
                        p.add("act", lambda e, hh=hh, pb=pb: e.activation(out=PT[r][:, hh * 4:(hh + 1) * 4, :], in_=PS[pb][:].rearrange("p (a b) -> p a b", a=4), func=AF.Copy),
                              reads=pk(pb), writes=[("PT", r, hh)])

                def pmc(blk):
                    r = blk % 2
                    b0 = 4 * r
                    for g in range(4):
                        pb = b0 + 2 + g // 2
                        lo = (g % 2) * 256
                        mm_group(PS[pb][:, lo:lo + 256],
                                 [(PT[r][:, 2 * g, :], WP[:, g, 0, :]), (PT[r][:, 2 * g + 1, :], WP[:, g, 1, :]), (ONES[0:1, :], PBR[0:1, g * 256:(g + 1) * 256])],
                                 reads=[("PT", r, g // 2), "WP", "ONES", "PBR"], writes=pk(pb, lo, lo + 256))
                    for half in range(2):
                        pb = b0 + 2 + half
                        p.add("dve", lambda e, half=half, pb=pb: e.tensor_tensor(out=TMPY[r][:, half * 512:(half + 1) * 512], in0=PS[pb][:, :], in1=BC[2][:, half * 512:(half + 1) * 512], op=ALU.mult),
                              reads=pk(pb) + [("BC", 2)], writes=[("TMPY", r)])
                    p.add("pool", lambda e: e.tensor_tensor(out=X[:, blk, :], in0=X[:, blk, :], in1=TMPY[r], op=ALU.add),
                          reads=[("TMPY", r)] + xk(blk), writes=xk(blk))

                for it in range(18):
                    if it < 16:
                        pma(it)
                    if 0 <= it - 1 < 16:
                        pmb(it - 1)
                    if 0 <= it - 2 < 16:
                        pmc(it - 2)
            while pending_casts and li == 0:
                cast_one(*pending_casts.pop(0))
            if stop_after == ("mix", li):
                break
            p.barrier()
            ar.reset()
            T1n = [ar.alloc(128, D, F32) for _ in range(3)]
            H2F = [ar.alloc(128, 8 * 128, F32).rearrange("p (a b) -> p a b", a=8) for _ in range(2)]
            load_bc(0, mods_row(li, s, 4), mods_keys(li, 4))
            load_bc(1, mods_row(li, s, 3), mods_keys(li, 3))
            rms_stats([(X[:, b, :], xk(b)) for b in range(16)], 1.0 / D)
            def n2a(blk):
                r3 = blk % 3
                p.add("dve", lambda e: e.scalar_tensor_tensor(
                    out=T1n[r3], in0=X[:, blk, :], scalar=RSTD[:, blk:blk + 1], in1=BC[0][:], op0=ALU.mult, op1=ALU.mult),
                    reads=xk(blk) + ["RSTD", ("BC", 0)], writes=[("T1", r3)])
                p.add("pool", lambda e: e.tensor_tensor(out=T1n[r3], in0=T1n[r3], in1=BC[1][:], op=ALU.add),
                      reads=[("T1", r3), ("BC", 1)], writes=[("T1", r3)])

            def n2b(blk):
                r3 = blk % 3
                r = blk % 2
                b0 = 4 * r
                for hh in range(2):
                    pb = b0 + hh
                    for kc4 in range(4):
                        kc = hh * 4 + kc4
                        mm_group(PS[pb][:, kc4 * 128:(kc4 + 1) * 128], [(T1n[r3][:, kc * 128:(kc + 1) * 128], IDF[:])],
                                 reads=[("T1", r3), "IDF"], writes=pk(pb, kc4 * 128, kc4 * 128 + 128))
                    p.add("act", lambda e, hh=hh, pb=pb: e.activation(out=HT[:, hh * 4:(hh + 1) * 4, blk * 128:(blk + 1) * 128],
                                                                      in_=PS[pb][:].rearrange("p (a b) -> p a b", a=4), func=AF.Copy),
                          reads=pk(pb), writes=[("HT", blk)] if hh == 0 else [("HTb", blk)])
                    p.add("dve", lambda e, hh=hh, pb=pb: e.tensor_copy(out=H2F[r][:, hh * 4:(hh + 1) * 4, :], in_=PS[pb][:].rearrange("p (a b) -> p a b", a=4)),
                          reads=pk(pb), writes=[("H2F", r, hh)])

            def n2c(blk):
                r = blk % 2
                b0 = 4 * r
                mm_group(PS[b0 + 2][:, 0:NE], [(H2F[r][:, kc, :], WR[:, kc, :]) for kc in range(8)],
                         reads=[("H2F", r, 0), ("H2F", r, 1), "WR"], writes=pk(b0 + 2, 0, 128))
                p.add("act", lambda e: e.activation(out=SC[:, blk, :], in_=PS[b0 + 2][:, 0:NE], func=AF.Sigmoid),
                      reads=pk(b0 + 2, 0, 128), writes=[("SC", blk)])

            for it in range(18):
                if it < 16:
                    n2a(it)
                if 0 <= it - 1 < 16:
                    n2b(it - 1)
                if 0 <= it - 2 < 16:
                    n2c(it - 2)
            sck = [("SC", b) for b in range(16)]
            SEL, CH, WT, _r3 = RT
            THR, M1, GS, TMPR = RS
            GM = SQT[:, 0:16]
            DEN = SSQ[:, 0:16]

            def dv(fn, reads, writes):
                p.add("dve", fn, reads=reads, writes=writes)
            dv(lambda e: e.tensor_tensor(out=SEL[:], in0=SC[:], in1=BRB[:], op=ALU.add), sck + brb_keys, ["SEL"])
            selv = SEL[:].rearrange("p b (g j) -> p (b g) j", j=4)
            a_ = [selv[:, :, j] for j in range(4)]
            pairs = [(0, 1), (0, 2), (0, 3), (1, 2), (1, 3), (2, 3)]
            for qi, (i0, i1) in enumerate(pairs):
                if qi == 0:
                    dv(lambda e, i0=i0, i1=i1: e.tensor_tensor(out=THR[:], in0=a_[i0], in1=a_[i1], op=ALU.min), ["SEL"], ["THR"])
                else:
                    dv(lambda e, i0=i0, i1=i1: e.tensor_tensor(out=TMPR[:], in0=a_[i0], in1=a_[i1], op=ALU.min), ["SEL"], ["TMPR"])
                    dv(lambda e: e.tensor_tensor(out=THR[:], in0=THR[:], in1=TMPR[:], op=ALU.max), ["THR", "TMPR"], ["THR"])
            dv(lambda e: e.tensor_reduce(out=M1[:], in_=selv, axis=AX.X, op=ALU.max), ["SEL"], ["M1"])
            dv(lambda e: e.tensor_tensor(out=GS[:], in0=M1[:], in1=THR[:], op=ALU.add), ["M1", "THR"], ["GS"])
            gsv = GS[:].rearrange("p (b g) -> p b g", g=4)
            dv(lambda e: e.tensor_reduce(out=GM, in_=gsv, axis=AX.X, op=ALU.max), ["GS"], ["GM"])
            isg = M1[:].rearrange("p (b g) -> p b g", g=4)
            dv(lambda e: e.tensor_tensor(out=isg, in0=gsv, in1=GM.unsqueeze(2).broadcast_to([128, 16, 4]), op=ALU.is_equal), ["GS", "GM", "M1"], ["ISG"])
            chv = CH[:].rearrange("p b (g j) -> p (b g) j", j=4)
            dv(lambda e: e.tensor_tensor(out=chv, in0=selv, in1=THR[:].unsqueeze(2).broadcast_to([128, 64, 4]), op=ALU.is_ge), ["SEL", "THR"], ["CH"])
            dv(lambda e: e.tensor_tensor(out=chv, in0=chv, in1=M1[:].unsqueeze(2).broadcast_to([128, 64, 4]), op=ALU.mult), ["CH", "ISG"], ["CH"])
            dv(lambda e: e.tensor_tensor(out=WT[:], in0=SC[:], in1=CH[:], op=ALU.mult), sck + ["CH"], ["WT"])
            dv(lambda e: e.tensor_reduce(out=DEN, in_=WT[:], axis=AX.X, op=ALU.add), ["WT"], ["DEN"])
            dv(lambda e: e.reciprocal(out=GM, in_=DEN), ["DEN", "GM"], ["RDEN"])
            dv(lambda e: e.tensor_tensor(out=GATES[:], in0=WT[:], in1=GM.unsqueeze(2).broadcast_to([128, 16, NE]), op=ALU.mult), ["WT", "RDEN"], ["GATES"])

            p.barrier()
            ar.reset()
            W1 = [ar.alloc(128, 8 * DE, BF16).rearrange("p (a b) -> p a b", a=8) for _ in range(2)]
            W3 = [ar.alloc(128, 8 * DE, BF16).rearrange("p (a b) -> p a b", a=8) for _ in range(2)]
            W2 = [ar.alloc(128, 4 * D, BF16).rearrange("p (a b) -> p a b", a=4) for _ in range(2)]
            HE = [ar.alloc(128, 4 * 512, BF16).rearrange("p (a b) -> p a b", a=4) for _ in range(2)]
            SG = [ar.alloc(128, 512, BF16) for _ in range(2)]
            G2B = ar.alloc(128, D, BF16)
            load_bc(0, mods_row(li, s, 5), mods_keys(li, 5))
            p.add("act", lambda e: e.activation(out=G2B, in_=BC[0][:], func=AF.Copy), reads=[("BC", 0)], writes=["G2B"])
            def moe_gu(ex, t, hb):
                wb = ex % 2
                if t == 0:
                    dma("sp", W1[wb], WG[li, ex].rearrange("(kc p) n -> p kc n", p=128), [("WG", li, ex)], [("W1", wb)], f"w1{wb}")
                    dma("sp", W3[wb], WU[li, ex].rearrange("(kc p) n -> p kc n", p=128), [("WU", li, ex)], [("W3", wb)], f"w3{wb}")
                    dma("sp", W2[wb], WD[li, ex].rearrange("(kc p) n -> p kc n", p=128), [("WD", li, ex)], [("W2", wb)], f"w2{wb}")
                    p.add("pool", lambda e: e.tensor_tensor(out=W2[wb], in0=W2[wb], in1=G2B.unsqueeze(1).broadcast_to([128, 4, D]), op=ALU.mult),
                          reads=[("W2", wb), "G2B"], writes=[("W2", wb)])
                htk = [("HT", b_) for b_ in range(t * 4, t * 4 + 4)] + [("HTb", b_) for b_ in range(t * 4, t * 4 + 4)]
                for m in range(4):
                    pg = m % 2
                    pu = 2 + m % 2
                    mm_group(PS[pg][:, :], [(W1[wb][:, kc, m * 128:(m + 1) * 128], HT[:, kc, t * 512:(t + 1) * 512]) for kc in range(8)],
                             reads=htk + [("W1", wb)], writes=pk(pg))
                    mm_group(PS[pu][:, :], [(W3[wb][:, kc, m * 128:(m + 1) * 128], HT[:, kc, t * 512:(t + 1) * 512]) for kc in range(8)],
                             reads=htk + [("W3", wb)], writes=pk(pu))
                    p.add("act", lambda e, pg=pg: e.activation(out=SG[pg], in_=PS[pg][:, :], func=AF.Silu), reads=pk(pg), writes=[("SG", pg)])
                    p.add("dve", lambda e, pg=pg, pu=pu, m=m: e.tensor_tensor(out=HE[hb][:, m, :], in0=PS[pu][:, :], in1=SG[pg], op=ALU.mult),
                          reads=pk(pu) + [("SG", pg)], writes=[("HE", hb, m)])

            def moe_down(ex, t, hb):
                wb = ex % 2
                for tb in range(4):
                    blk = t * 4 + tb
                    for half in range(2):
                        pd = 4 + (tb * 2 + half) % 4
                        mm_group(PS[pd][:, :], [(HE[hb][:, mc, tb * 128:(tb + 1) * 128], W2[wb][:, mc, half * 512:(half + 1) * 512]) for mc in range(4)],
                                 reads=[("HE", hb, m) for m in range(4)] + [("W2", wb)], writes=pk(pd))
                        p.add("dve", lambda e, pd=pd, blk=blk, half=half: e.scalar_tensor_tensor(
                            out=X[:, blk, half * 512:(half + 1) * 512], in0=PS[pd][:, :], scalar=GATES[:, blk, ex:ex + 1],
                            in1=X[:, blk, half * 512:(half + 1) * 512], op0=ALU.mult, op1=ALU.add),
                            reads=pk(pd) + ["GATES"] + xk(blk, half), writes=xk(blk, half))

            iters = [(ex, t) for ex in range(NE) for t in range(4)]
            for q in range(len(iters) + 1):
                if q < len(iters):
                    moe_gu(iters[q][0], iters[q][1], q % 2)
                if q >= 1:
                    moe_down(iters[q - 1][0], iters[q - 1][1], (q - 1) % 2)
            if stop_after == ("layer", li):
                break
        p.barrier()
        ar.reset()
        T1f = [ar.alloc(128, D, F32) for _ in range(4)]
        if do_final and stop_after is None:
            load_bc(0, final_g.partition_broadcast(128), [])
            rms_stats([(X[:, b, :], xk(b)) for b in range(16)], 1.0 / D)
            for blk in range(16):
                r = blk % 4
                p.add("dve", lambda e, r=r, blk=blk: e.scalar_tensor_tensor(
                    out=T1f[r], in0=X[:, blk, :], scalar=RSTD[:, blk:blk + 1], in1=BC[0][:], op0=ALU.mult, op1=ALU.mult),
                    reads=xk(blk) + ["RSTD", ("BC", 0)], writes=[("T1", r)])
                dma("sp", out4[s, blk * 128:(blk + 1) * 128, :], T1f[r], [("T1", r)], [("OUT", s, blk)], f"o{r}")
        else:
            for g4 in range(4):
                dma("sp", out4[s, g4 * 512:(g4 + 1) * 512, :].rearrange("(b p) d -> p b d", p=128), X[:, g4 * 4:(g4 + 1) * 4, :],
                    [k for b in range(g4 * 4, g4 * 4 + 4) for k in xk(b)], [("OUT", s, g4)], "od", 4)
    p.barrier()
    p.finalize_and_emit()
    st.close()
    return nc, p


import os
DBG_A = int(os.environ.get('K_DBG_A', 9))
DBG_B = int(os.environ.get('K_DBG_B', 9))
_CACHE = {}


def _run(inputs, layers, do_final, nseq=SEQ_PER_CORE, stop_after=None, x_override=None, ncores=NCORES):
    key = (tuple(layers), do_final, nseq, stop_after)
    if key not in _CACHE:
        _CACHE[key] = build_program(layers, do_final, nseq, stop_after)
    nc, _ = _CACHE[key]
    cst = _consts()
    f = lambda a: np.ascontiguousarray(np.asarray(a, dtype=np.float32))
    x = f(inputs["x"]) if x_override is None else x_override
    c = f(inputs["c"])
    ctx = f(inputs["ctx"])
    c_ctx = f(inputs["c_ctx"])
    shared = dict(
        norm1_g=f(inputs["norm1_g"]), norm2_g=f(inputs["norm2_g"]), w_mod=f(inputs["w_mod"]), b_mod=f(inputs["b_mod"]),
        gla_w_in=f(inputs["gla_w_in"])[0], gla_w_gate_a=f(inputs["gla_w_gate_a"])[0], gla_w_gate_b=f(inputs["gla_w_gate_b"])[0],
        gla_b_gate=f(inputs["gla_b_gate"])[0], gla_norm_g=f(inputs["gla_norm_g"]), gla_w_out=f(inputs["gla_w_out"])[0],
        pool_w=f(inputs["pool_w"])[0], pool_b=f(inputs["pool_b"]).reshape(1, D), pool_scale=f(inputs["pool_scale"]).reshape(1, D),
        w_router=f(inputs["w_router"]), b_router=f(inputs["b_router"]).reshape(1, NE),
        w_gate_e=f(inputs["w_gate_e"]), w_up_e=f(inputs["w_up_e"]), w_down_e=f(inputs["w_down_e"]),
        final_g=f(inputs["final_g"]).reshape(1, D), **cst)
    in_maps = []
    for k in range(ncores):
        b0 = k * nseq
        m = dict(shared)
        m["x4"] = np.ascontiguousarray(x[b0:b0 + nseq])
        m["ctx4"] = np.ascontiguousarray(ctx[b0:b0 + nseq])
        cc = np.concatenate([c[b0:b0 + nseq], np.zeros((4 - nseq, D), np.float32), c_ctx[None, :]], axis=0)
        m["ccT"] = np.ascontiguousarray(cc.T)
        in_maps.append(m)
    res = run_bass_kernel_spmd(nc, in_maps, core_ids=list(range(ncores)))
    return np.concatenate([np.asarray(r["out4"]) for r in res.results], axis=0)


def kernel(**inputs):
    return _run(inputs, (0, 1), True).astype(np.float32)
```
